# Optimizing a Trainium2 kernel written in Bass

```python
import jax, jax.numpy as jnp
from jax import lax
import numpy as np

D_MODEL = 1024
BATCH = 8
SEQ = 2048
DEPTH = 1

GRID_W = 64
CTX_LEN = 256
D_MIX = D_MODEL
D_SGU = D_MIX // 2
SGU_HEADS = 4
SGU_HEAD_DIM = D_SGU // SGU_HEADS
CHUNK = 128
D_LRU = D_MIX - D_SGU
LRU_HEADS = 8
LRU_HEAD_DIM = D_LRU // LRU_HEADS
CONV_W = 4
CONV_PAD = (1, 2)
RG_C = 8.0
N_EXPERTS = 16
EC_FACTOR = 2
D_EXPERT = 2048
N_MOD = 6
D_IN = 2 * D_SGU + 2 * D_LRU
EPS = 1e-6

kernel_name = "hybrid_sgu_rglru_ecmoe_dit"


def rmsnorm(x, g):
    xf = x.astype(jnp.float32)
    y = xf * lax.rsqrt(jnp.mean(xf * xf, axis=-1, keepdims=True) + EPS)
    return (y * g.astype(jnp.float32)).astype(x.dtype)


def spatial_gating(u, v, g, w_s, b_s):
    bn, L, _ = u.shape
    u = jax.nn.gelu(u)
    vh = jax.nn.gelu(v).reshape(bn, L, SGU_HEADS, SGU_HEAD_DIM)
    vh = rmsnorm(vh, g.reshape(SGU_HEADS, SGU_HEAD_DIM))
    vc = vh.reshape(bn, L // CHUNK, CHUNK, SGU_HEADS, SGU_HEAD_DIM)
    s = jnp.einsum('hpq,bnqhc->bnphc', w_s, vc) + jnp.swapaxes(b_s, 0, 1)[None, None, :, :, None]
    return u * s.reshape(bn, L, D_SGU)


def depthwise_conv(x, w, b):
    ch = x.shape[-1]
    y = lax.conv_general_dilated(x, w[:, None, :].astype(x.dtype), window_strides=(1,),
                                 padding=[CONV_PAD], dimension_numbers=('NWC', 'WIO', 'NWC'),
                                 feature_group_count=ch)
    return y + b


def rglru_coeffs(xb, wa, ba, wi, bi, lam):
    bn, L, _ = xb.shape
    xh = xb.reshape(bn, L, LRU_HEADS, LRU_HEAD_DIM)
    r = jax.nn.sigmoid(jnp.einsum('blhi,hij->blhj', xh, wa).reshape(bn, L, D_LRU) + ba)
    i = jax.nn.sigmoid(jnp.einsum('blhi,hij->blhj', xh, wi).reshape(bn, L, D_LRU) + bi)
    log_a = RG_C * r.astype(jnp.float32) * jax.nn.log_sigmoid(lam.astype(jnp.float32))
    a = jnp.exp(log_a)
    drive = jnp.sqrt(-jnp.expm1(2.0 * log_a)) * (i * xb).astype(jnp.float32)
    return a, drive


def linear_scan(a, b, h0):
    if h0 is not None:
        b = b.at[:, 0].add(a[:, 0] * h0)

    def combine(left, right):
        a_l, b_l = left
        a_r, b_r = right
        return a_l * a_r, a_r * b_l + b_r

    _, h = lax.associative_scan(combine, (a, b), axis=1)
    return h


def token_mixer(hx, hc, w_in, sgu_g, sgu_w, sgu_b, conv_w, conv_b,
                rg_wa, rg_ba, rg_wi, rg_bi, rg_lam, w_out, ctx_out):
    bn, n, _ = hx.shape
    rows = n // GRID_W
    splits = [D_SGU, 2 * D_SGU, 2 * D_SGU + D_LRU]
    ux, vx, xx, gx = jnp.split(hx @ w_in, splits, axis=-1)
    uc, vc, xc, gc = jnp.split(hc @ w_in, splits, axis=-1)

    cx = depthwise_conv(xx.reshape(bn * rows, GRID_W, D_LRU), conv_w, conv_b).reshape(bn, n, D_LRU)
    cc = depthwise_conv(xc, conv_w, conv_b)

    a_cf, b_cf = rglru_coeffs(cc, rg_wa[0], rg_ba[0], rg_wi[0], rg_bi[0], rg_lam[0])
    h_cf = linear_scan(a_cf, b_cf, None)
    a_xf, b_xf = rglru_coeffs(cx, rg_wa[0], rg_ba[0], rg_wi[0], rg_bi[0], rg_lam[0])
    h_xf = linear_scan(a_xf, b_xf, h_cf[:, -1])

    a_cb, b_cb = rglru_coeffs(jnp.flip(cc, 1), rg_wa[1], rg_ba[1], rg_wi[1], rg_bi[1], rg_lam[1])
    h_cb = linear_scan(a_cb, b_cb, None)
    a_xb, b_xb = rglru_coeffs(jnp.flip(cx, 1), rg_wa[1], rg_ba[1], rg_wi[1], rg_bi[1], rg_lam[1])
    h_xb = jnp.flip(linear_scan(a_xb, b_xb, h_cb[:, -1]), 1)

    rec_x = jax.nn.gelu(gx) * (h_xf + h_xb).astype(hx.dtype)
    yx = jnp.concatenate([spatial_gating(ux, vx, sgu_g, sgu_w, sgu_b), rec_x], axis=-1) @ w_out
    if ctx_out:
        rec_c = jax.nn.gelu(gc) * (h_cf + jnp.flip(h_cb, 1)).astype(hc.dtype)
        yc = jnp.concatenate([spatial_gating(uc, vc, sgu_g, sgu_w, sgu_b), rec_c], axis=-1) @ w_out
    else:
        yc = None
    return yx, yc


def expert_choice_ffn(h, w_r, w1, w3, w2):
    bn, n, _ = h.shape
    cap = EC_FACTOR * n // N_EXPERTS
    aff = jax.nn.softmax((h @ w_r).astype(jnp.float32), axis=-1)
    gate, idx = lax.top_k(jnp.swapaxes(aff, 1, 2), cap)
    bidx = jnp.arange(bn)[:, None, None]
    xe = h[bidx, idx]
    hid = jax.nn.silu(jnp.einsum('becd,edf->becf', xe, w1)) * jnp.einsum('becd,edf->becf', xe, w3)
    ye = jnp.einsum('becf,efd->becd', hid, w2) * gate[..., None].astype(h.dtype)
    return jnp.zeros_like(h).at[bidx, idx].add(ye)


def setup_inputs(seed: int = 0) -> dict:
    key = jax.random.key(seed)
    ks = jax.random.split(key, 32)
    f32 = jnp.float32

    def nrm(k, shape, scale):
        return jax.random.normal(k, shape, f32) * scale

    a0 = jax.random.uniform(ks[20], (DEPTH, 2, D_LRU), f32, 0.9, 0.999)
    p = a0 ** (1.0 / RG_C)
    rg_lam = jnp.log(p) - jnp.log1p(-p)
    return {
        "x": nrm(ks[0], (BATCH, SEQ, D_MODEL), 1.0),
        "c": nrm(ks[1], (BATCH, D_MODEL), 1.0),
        "ctx": nrm(ks[2], (BATCH, CTX_LEN, D_MODEL), 1.0),
        "c_ctx": nrm(ks[3], (D_MODEL,), 1.0),
        "w_mod": nrm(ks[4], (DEPTH, D_MODEL, N_MOD * D_MODEL), 0.5 * D_MODEL ** -0.5),
        "b_mod": nrm(ks[5], (DEPTH, N_MOD * D_MODEL), 0.02),
        "norm1_g": 1.0 + nrm(ks[6], (DEPTH, D_MODEL), 0.02),
        "norm2_g": 1.0 + nrm(ks[7], (DEPTH, D_MODEL), 0.02),
        "w_in": nrm(ks[8], (DEPTH, D_MODEL, D_IN), D_MODEL ** -0.5),
        "sgu_g": 1.0 + nrm(ks[9], (DEPTH, D_SGU), 0.02),
        "sgu_w": nrm(ks[10], (DEPTH, SGU_HEADS, CHUNK, CHUNK), CHUNK ** -0.5),
        "sgu_b": 1.0 + nrm(ks[11], (DEPTH, SGU_HEADS, CHUNK), 0.02),
        "conv_w": nrm(ks[12], (DEPTH, CONV_W, D_LRU), CONV_W ** -0.5),
        "conv_b": nrm(ks[13], (DEPTH, D_LRU), 0.02),
        "rg_wa": nrm(ks[14], (DEPTH, 2, LRU_HEADS, LRU_HEAD_DIM, LRU_HEAD_DIM), LRU_HEAD_DIM ** -0.5),
        "rg_ba": nrm(ks[15], (DEPTH, 2, D_LRU), 0.02),
        "rg_wi": nrm(ks[16], (DEPTH, 2, LRU_HEADS, LRU_HEAD_DIM, LRU_HEAD_DIM), LRU_HEAD_DIM ** -0.5),
        "rg_bi": nrm(ks[17], (DEPTH, 2, D_LRU), 0.02),
        "rg_lam": rg_lam,
        "w_out": nrm(ks[18], (DEPTH, D_MIX, D_MODEL), D_MIX ** -0.5),
        "w_router": nrm(ks[19], (DEPTH, D_MODEL, N_EXPERTS), D_MODEL ** -0.5),
        "w1": nrm(ks[21], (DEPTH, N_EXPERTS, D_MODEL, D_EXPERT), D_MODEL ** -0.5),
        "w3": nrm(ks[22], (DEPTH, N_EXPERTS, D_MODEL, D_EXPERT), D_MODEL ** -0.5),
        "w2": nrm(ks[23], (DEPTH, N_EXPERTS, D_EXPERT, D_MODEL), D_EXPERT ** -0.5),
        "final_g": 1.0 + nrm(ks[24], (D_MODEL,), 0.02),
    }


def reference(x, c, ctx, c_ctx, w_mod, b_mod, norm1_g, norm2_g, w_in, sgu_g, sgu_w, sgu_b,
              conv_w, conv_b, rg_wa, rg_ba, rg_wi, rg_bi, rg_lam, w_out, w_router, w1, w3, w2,
              final_g):
    h_ctx = ctx
    for l in range(DEPTH):
        last = l == DEPTH - 1
        mod_x = jax.nn.silu(c) @ w_mod[l] + b_mod[l]
        mod_c = jax.nn.silu(c_ctx) @ w_mod[l] + b_mod[l]
        sh1x, sc1x, g1x, sh2x, sc2x, g2x = jnp.split(mod_x[:, None, :], N_MOD, axis=-1)
        sh1c, sc1c, g1c, sh2c, sc2c, g2c = jnp.split(mod_c, N_MOD, axis=-1)

        hx = rmsnorm(x, norm1_g[l]) * (1.0 + sc1x) + sh1x
        hc = rmsnorm(h_ctx, norm1_g[l]) * (1.0 + sc1c) + sh1c
        yx, yc = token_mixer(hx, hc, w_in[l], sgu_g[l], sgu_w[l], sgu_b[l], conv_w[l], conv_b[l],
                             rg_wa[l], rg_ba[l], rg_wi[l], rg_bi[l], rg_lam[l], w_out[l],
                             not last)
        x = x + g1x * yx
        hx2 = rmsnorm(x, norm2_g[l]) * (1.0 + sc2x) + sh2x
        x = x + g2x * expert_choice_ffn(hx2, w_router[l], w1[l], w3[l], w2[l])

        if not last:
            h_ctx = h_ctx + g1c * yc
            hc2 = rmsnorm(h_ctx, norm2_g[l]) * (1.0 + sc2c) + sh2c
            h_ctx = h_ctx + g2c * expert_choice_ffn(hc2, w_router[l], w1[l], w3[l], w2[l])
    return rmsnorm(x, final_g)
```

```python
import numpy as np
import concourse.bass as bass
import concourse.mybir as mybir
from concourse.bass_utils import run_bass_kernel_spmd

F32 = mybir.dt.float32
BF16 = mybir.dt.bfloat16
I32 = mybir.dt.int32
AF = mybir.ActivationFunctionType
ALU = mybir.AluOpType
AX = mybir.AxisListType

NCORES = 8
S = 2048
D = 1024
CTX = 256
NE = 16
CAP = 256
EPS = 1e-6
XROW = 1072
NSLOT_LO = 8
NSLOT = 14
SLOT_BYTES = 8192
NBISECT = 28
SAME_ENGINE_SYNC = True
import os as _os
EVAC_ACT_ONLY = _os.environ.get('EVAC_ACT_ONLY', '0') == '1'

_ESZ = {F32: 4, BF16: 2, I32: 4}


class Buf:
    def __init__(self, t, space, off, shape, dt):
        self.t, self.space, self.off, self.shape, self.dt = t, space, off, shape, dt
        self.esz = _ESZ[dt]
        n = 1
        for s in shape[1:]:
            n *= s
        self.nbytes = n * self.esz

    def __getitem__(self, k):
        return self.t[k]

    def r(self, lo=None, hi=None):
        lo = 0 if lo is None else lo
        hi = (self.nbytes // self.esz) if hi is None else hi
        return (self.space, self.off + lo * self.esz, self.off + hi * self.esz)

    def r3(self, k, c0, c1):
        n = self.shape[-1]
        return self.r(k * n + c0, k * n + c1)

    def rks(self, ks, c0, c1):
        return [self.r3(k, c0, c1) for k in ks]


class Op:
    __slots__ = ("eng", "fn", "deps", "is_dma", "sem", "val", "signal", "cnt", "grp")


class Prog:
    GRAN = 64

    def __init__(self, nc):
        self.nc = nc
        self.ops = []
        self.by_eng = {e: [] for e in ("pe", "act", "dve", "pool", "sp")}
        self.blocks = {}
        self.keys = {}
        self.dma_groups = {}

    def _state(self, key):
        st = self.blocks.get(key)
        if st is None:
            st = [None, {}, []]
            self.blocks[key] = st
        return st

    def _keys_of(self, rng):
        if rng[0] == "dram":
            return [rng]
        space, lo, hi = rng
        gran = 2048 if space == "ps" else self.GRAN
        return [(space, b) for b in range(lo // gran, (hi - 1) // gran + 1)]

    def add(self, eng, fn, reads=(), writes=(), dma=None):
        op = Op()
        op.eng, op.fn, op.is_dma, op.signal, op.cnt, op.grp = eng, fn, dma is not None, False, None, dma
        oid = len(self.ops)
        deps = set()
        rkeys = []
        wkeys = []
        for rng in reads:
            (wkeys if rng[0] == "ps" else rkeys).extend(self._keys_of(rng))
        for rng in writes:
            wkeys.extend(self._keys_of(rng))
        for k in rkeys:
            st = self._state(k)
            if st[0] is not None:
                deps.add(st[0])
        for k in wkeys:
            st = self._state(k)
            if st[0] is not None:
                deps.add(st[0])
            deps.update(st[1].values())
            deps.update(st[2])
        for k in rkeys:
            st = self._state(k)
            if op.is_dma:
                st[2].append(oid)
            else:
                st[1][eng] = oid
        for k in wkeys:
            st = self._state(k)
            st[0] = oid
            st[1] = {}
            st[2] = []
        deps.discard(oid)
        best = {}
        final = []
        for d in deps:
            p = self.ops[d]
            if p.is_dma:
                final.append(d)
            else:
                if p.eng == eng and not op.is_dma:
                    if eng == "pe" or not SAME_ENGINE_SYNC:
                        continue
                if p.eng not in best or best[p.eng] < d:
                    best[p.eng] = d
        final.extend(best.values())
        for d in final:
            self.ops[d].signal = True
        op.deps = final
        if op.is_dma:
            g = self.dma_groups.setdefault(dma, {"sem": None, "n": 0})
            g["n"] += 1
            op.val = 16 * g["n"]
            op.signal = True
        self.ops.append(op)
        self.by_eng[eng].append(oid)
        return oid

    def emit(self, stack):
        nc = self.nc
        esem = {e: stack.enter_context(nc.semaphore("sem_" + e)) for e in ("pe", "act", "dve", "pool")}
        for name, g in self.dma_groups.items():
            g["sem"] = stack.enter_context(nc.semaphore("dma_" + name))
        for e, lst in self.by_eng.items():
            c = 0
            for oid in lst:
                op = self.ops[oid]
                if op.is_dma:
                    continue
                if op.signal:
                    c += 1
                    op.cnt = c
        ops = self.ops
        groups = self.dma_groups

        def token(p):
            if p.is_dma:
                g = groups[p.grp]
                v = 16 * g["n"] if p.grp.startswith("T:") else p.val
                return g["sem"], v
            return esem[p.eng], p.cnt

        def run(engname, eng):
            waited = {}
            lst = self.by_eng[engname]
            for oid in lst:
                op = ops[oid]
                for d in op.deps:
                    sem, v = token(ops[d])
                    key = id(sem)
                    if waited.get(key, 0) >= v:
                        continue
                    waited[key] = v
                    eng.wait_ge(sem, v)
                ins = op.fn(eng)
                if op.is_dma:
                    ins.then_inc(groups[op.grp]["sem"], 16)
                elif op.signal:
                    ins.then_inc(esem[engname], 1)
            for name, g in groups.items():
                pass
            return waited

        block = stack.enter_context(nc.Block())

        @block.tensor
        def _(e):
            run("pe", e)

        @block.scalar
        def _(e):
            run("act", e)

        @block.vector
        def _(e):
            run("dve", e)

        @block.gpsimd
        def _(e):
            run("pool", e)

        @block.sync
        def _(e):
            run("sp", e)
            for name, g in groups.items():
                e.wait_ge(g["sem"], 16 * g["n"])
            for en in ("pe", "act", "dve", "pool"):
                c = 0
                for oid in self.by_eng[en]:
                    if ops[oid].cnt:
                        c = ops[oid].cnt
                if c:
                    e.wait_ge(esem[en], c)


def build_program(debug=False, stop=None):
    from contextlib import ExitStack
    nc = bass.Bass("TRN2", target_bir_lowering=False)
    P = Prog(nc)

    def finish():
        from contextlib import ExitStack as _ES
        with _ES() as stack:
            P.emit(stack)
        return nc

    def din(name, shape, dt=F32):
        return nc.dram_tensor(name, list(shape), dt, kind="ExternalInput").ap()

    x_d = din("x", [S, D])
    ctx_d = din("ctx", [CTX, D])
    cvec_d = din("cvec", [128, 8, 2])
    wmod_d = din("w_mod", [D, 6 * D])
    bmod_d = din("bmod", [128, 48])
    n1g_d = din("n1g", [128, 8])
    n2g_d = din("n2g", [128, 8])
    win_d = din("w_in", [D, 2048])
    sgug_d = din("sgug_rep", [128, 512])
    wsT_d = din("wsT", [128, 4, 128])
    sgub_d = din("sgub", [1, 512])
    convw_d = din("convw", [128, 4, 4])
    convb_d = din("convb", [128, 4])
    wg_d = din("wg", [128, 16, 128])
    gb_d = din("gb", [128, 16])
    lam_d = din("lam", [128, 8])
    wout_d = din("w_out", [D, D])
    wr_d = din("wr", [128, 8, 16])
    w1_d = din("w1", [NE, D, 2048])
    w3_d = din("w3", [NE, D, 2048])
    w2_d = din("w2", [NE, 2048, D])
    fg_d = din("fg_rep", [128, D])
    ident_d = din("ident", [128, 128])
    iota_d = din("iota256", [128, 256])
    ltri_d = din("ltri", [128, 128])
    blk_d = din("blk", [128, 128])
    self_d = din("selfull", [128, 256])
    tinfo_d = din("tinfo", [128, 16, 2])
    out_d = nc.dram_tensor("out", [S, D], F32, kind="ExternalOutput").ap()
    xn2_dram = nc.dram_tensor("xn2_scratch", [S, XROW], BF16, kind="Internal").ap()
    acc_dram = nc.dram_tensor("acc_scratch", [S, D], F32, kind="Internal").ap()
    dbg = {}
    if debug:
        dbg["mixT"] = nc.dram_tensor("dbg_mixT", [128, 8, S], BF16, kind="ExternalOutput").ap()
        dbg["aff"] = nc.dram_tensor("dbg_aff", [128, 16, 16], F32, kind="ExternalOutput").ap()
        dbg["rankm"] = nc.dram_tensor("dbg_rankm", [128, 256], F32, kind="ExternalOutput").ap()
        dbg["mod"] = nc.dram_tensor("dbg_mod", [128, 48, 2], F32, kind="ExternalOutput").ap()
        dbg["x1"] = nc.dram_tensor("dbg_x1", [S, D], F32, kind="ExternalOutput").ap()

    def sb(name, shape, dt, off):
        t = nc.alloc_sbuf_tensor_at(name, list(shape), dt, offset=off)
        return Buf(t, "sb", off, shape, dt)

    class Bump:
        def __init__(self, lo, hi):
            self.p, self.hi = lo, hi

        def __call__(self, name, shape, dt, align=64):
            n = 1
            for s in shape[1:]:
                n *= s
            nb = n * _ESZ[dt]
            self.p = (self.p + align - 1) // align * align
            off = self.p
            self.p += nb
            assert self.p <= self.hi, (name, self.p, self.hi)
            return sb(name, shape, dt, off)

    base = (int(nc.sbuf_base) + 63) // 64 * 64
    arena = nc.alloc_sbuf_tensor("arena", [128, 212800], mybir.dt.uint8)
    OFF_POOL = base
    OFF_MIX = OFF_POOL + NSLOT_LO * SLOT_BYTES
    OFF_HX = OFF_MIX + 32768
    OFF_RG = OFF_HX + 32768
    OFF_MISC = OFF_RG + 51200
    END = base + 212800

    slotA, slotB = [], []
    for s in range(NSLOT):
        if s < NSLOT_LO:
            off = OFF_POOL + s * SLOT_BYTES
        elif s < 12:
            off = OFF_HX + (s - NSLOT_LO) * SLOT_BYTES
        else:
            off = OFF_MISC + (s - 12) * SLOT_BYTES
        slotA.append(sb(f"slotA{s}", [128, 8, 512], BF16, off))
        slotB.append(sb(f"slotB{s}", [128, 4, 1024], BF16, off))

    mixT = sb("mixT", [128, 8, S], BF16, OFF_MIX)
    hxT = sb("hxT", [128, 8, S], BF16, OFF_HX)

    rgb = Bump(OFF_RG, OFF_MISC)
    xxp = rgb("xxp", [128, 32, 67], F32)
    xcp = rgb("xcp", [128, 259], F32)
    cx = rgb("cx", [128, 2304], F32)
    cxb = rgb("cxb", [128, 2304], BF16)
    Rb = rgb("Rb", [128, 2304], F32)
    Sb = rgb("Sb", [128, 2304], F32)
    Ib = rgb("Ib", [128, 2304], F32)
    Hf = sb("Hf", [128, 2304], F32, xxp.off)
    vb = Bump(Rb.off, OFF_MISC)
    gvs = [vb(f"gv{i}", [128, 512], F32) for i in range(4)]
    gsqfs = [vb(f"gsqf{i}", [128, 512], F32) for i in range(2)]
    ss4a = vb("ss4a", [128, 16, 4], F32)
    r4a = vb("r4a", [128, 16, 4], F32)
    vn = [vb(f"vn{i}", [128, 512], BF16) for i in range(2)]
    wb = Bump(Sb.off, OFF_MISC)
    g1x_rep = wb("g1x_rep", [128, D], F32)
    dg = wb("dg", [128, 128], F32)

    mb = Bump(OFF_MISC, END)
    hcT = mb("hcT", [128, 8, CTX], BF16)
    xt = [mb(f"xt{i}", [128, D], F32) for i in range(2)]
    xnb = [mb(f"xnb{i}", [128, D], BF16) for i in range(2)]
    identb = mb("identb", [128, 128], BF16)
    identf = mb("identf", [128, 128], F32)
    onesb = mb("onesb", [128, 128], BF16)
    onesf = mb("onesf", [128, 128], F32)
    wgb = mb("wgb", [128, 16, 128], BF16)
    wsTb = mb("wsTb", [128, 4, 128], BF16)
    sgug = mb("sgug", [128, 512], F32)
    sgubb = mb("sgubb", [1, 512], BF16)
    modsb = mb("modsb", [128, 48, 2], F32)
    cvec = mb("cvec", [128, 8, 2], F32)
    scb = mb("scb", [128, 8, 2], BF16)
    bmod = mb("bmod", [128, 48], F32)
    n1g = mb("n1g", [128, 8], F32)
    n2g = mb("n2g", [128, 8], F32)
    gsc1x = mb("gsc1x", [128, 8], F32)
    gsc1c = mb("gsc1c", [128, 8], F32)
    gsc2x = mb("gsc2x", [128, 8], F32)
    convw = mb("convw", [128, 4, 4], F32)
    convb = mb("convb", [128, 4], F32)
    gbv = mb("gbv", [128, 16], F32)
    lam = mb("lam", [128, 8], F32)
    cl = mb("cl", [128, 8], F32)
    cl2 = mb("cl2", [128, 8], F32)
    ss = mb("ss", [128, 18], F32)
    rs = mb("rs", [128, 18], F32)
    rstd = mb("rstd", [128, 18], F32)
    ss4 = mb("ss4", [128, 4], F32)
    r4 = mb("r4", [128, 4], F32)
    wrf = mb("wrf", [128, 8, 16], F32)
    wrp = mb("wrp", [128, 8, 16], BF16)
    wrb = mb("wrb", [128, 8, 16], BF16)
    sh2b = mb("sh2b", [128, 8], BF16)
    rbiasb = mb("rbiasb", [1, 16], BF16)
    ss2 = mb("ss2", [128, 16], F32)
    rs2 = mb("rs2", [128, 16], F32)
    rstd2 = mb("rstd2", [128, 16], F32)
    smx = mb("smx", [128, 4], F32)
    idx_all = mb("idx_all", [128, NE, 2], I32)
    lo_t = mb("lo_t", [128, 1], F32)
    neghalf = mb("neghalf", [128, 16], F32)
    idxf = mb("idxf", [128, 2], F32)
    mid_t = mb("mid_t", [128, 1], F32)
    cnt2 = mb("cnt2", [128, 2], F32)
    stp = mb("stp", [128, 1], F32)
    ss3 = mb("ss3", [128, 16], F32)
    rs3 = mb("rs3", [128, 16], F32)
    rstd3 = mb("rstd3", [128, 16], F32)

    xb = Bump(OFF_MIX, OFF_HX)
    Se = xb("Se", [128, 16, 256], BF16)
    hidT = xb("hidT", [128, 16, 256], BF16)
    ye = xb("ye", [128, 2, D], F32)
    xeT = xb("xeT", [128, 8, 256], BF16)
    sl = [xb(f"sl{i}", [128, 256], F32) for i in range(2)]
    eb = Bump(OFF_RG, OFF_MISC)
    xg = eb("xg", [128, 2, XROW], BF16)
    g2x_rep = eb("g2x_rep", [128, D], F32)
    fg_rep = eb("fg_rep", [128, D], F32)
    iota256 = eb("iota256", [128, 256], F32)
    rankm = eb("rankm", [128, 256], F32)
    aff_all = eb("aff_all", [128, 16, 16], F32)
    maskf = eb("maskf", [128, 256], F32)
    maskb = eb("maskb", [128, 256], BF16)
    offt = eb("offt", [128, 16, 16], F32)
    rk = eb("rk", [128, 256], F32)
    affg = eb("affg", [128, 2, 128], F32)
    affT = eb("affT", [128, 256], F32)
    cmpj = eb("cmpj", [128, 256], BF16)
    selt = eb("selt", [128, 256], F32)
    ltrib = eb("ltrib", [128, 128], BF16)
    blkf = eb("blkf", [128, 128], F32)
    selfull = eb("selfull", [128, 256], F32)
    tinfob = eb("tinfob", [128, 16, 2], BF16)
    exl = eb("exl", [128, 16], F32)
    x1t = [eb(f"x1t{i}", [128, D], F32) for i in range(2)]
    xn2b = [eb(f"xn2b{i}", [128, D], BF16) for i in range(2)]
    xn2Ts = [eb(f"xn2T{i}", [128, 8, 128], BF16) for i in range(2)]
    exls = [eb(f"exl{i}", [128, 16], F32) for i in range(2)]
    smxa = eb("smxa", [128, 16, 4], F32)
    tail_all = eb("tail_all", [128, 16, 48], BF16)
    dg2 = eb("dg2", [128, 128], F32)
    ob = Bump(OFF_MIX, OFF_HX)
    outt = [ob(f"outt{i}", [128, D], F32) for i in range(4)]
    x2e = [sb(f"x2e{i}", [128, D], F32, ye.off + i * 4096) for i in range(2)]

    def ps(name, shape, dt):
        t = nc.alloc_psum_tensor(name, list(shape), dt)
        return t

    pbank = []
    for i in range(6):
        t = ps(f"pb{i}", [128, 512], F32)
        pbank.append(Buf(t, "ps", i * 2048, [128, 512], F32))
    for b_ in pbank:
        b_.bf = b_.t[:].bitcast(BF16)
    tbanks = pbank[0:4]
    trps = Buf(ps("trps", [128, 8, 128], BF16), "ps", 6 * 2048, [128, 8, 128], BF16)
    pmisc = Buf(ps("pmisc", [128, 512], F32), "ps", 7 * 2048, [128, 512], F32)

    def dma(eng, grp, out_ap, in_ap, reads, writes):
        return P.add(eng, lambda e, o=out_ap, i=in_ap: e.dma_start(out=o, in_=i), reads, writes, dma=grp)

    CONST = "T:const"
    CONSTP = "T:constp"

    def load_const(buf, src):
        dma("sp", CONST, buf[:], src, [], [buf.r()])

    def load_const_cast(buf, src):
        dma("pool", CONSTP, buf[:], src, [], [buf.r()])

    units = []

    def rA(ap):
        return ap.rearrange("(k p) n -> p k n", p=128)

    for j in range(4):
        units.append((rA(wmod_d[:, j * 512:(j + 1) * 512]), "A"))
    for c in (0, 1, 3, 2):
        units.append((rA(win_d[:, c * 512:(c + 1) * 512]), "A"))
    for j in range(4, 12):
        units.append((rA(wmod_d[:, j * 512:(j + 1) * 512]), "A"))
    for n in range(2):
        units.append((rA(wout_d[:, n * 512:(n + 1) * 512]), "A"))
    for e in range(NE):
        for cb in range(4):
            units.append((rA(w1_d[e][:, cb * 512:(cb + 1) * 512]), "A"))
            units.append((rA(w3_d[e][:, cb * 512:(cb + 1) * 512]), "A"))
        for kg in range(4):
            units.append((rA(w2_d[e][kg * 512:(kg + 1) * 512, :]), "B"))

    st = {"next": 0, "free": list(range(NSLOT_LO)), "acq": 0, "slot_of": {}, "hi_open": False}

    def pump():
        while st["next"] < len(units) and st["free"]:
            s = st["free"].pop(0)
            u = st["next"]
            st["next"] += 1
            src, kind = units[u]
            buf = slotA[s] if kind == "A" else slotB[s]
            dma("pool", f"slot{s}", buf[:], src, [], [buf.r()])
            st["slot_of"][u] = s

    def acquire():
        u = st["acq"]
        st["acq"] += 1
        if u not in st["slot_of"]:
            pump()
        s = st["slot_of"][u]
        kind = units[u][1]
        return s, (slotA[s] if kind == "A" else slotB[s])

    def release(s):
        st["free"].append(s)
        pump()

    def open_hi_slots():
        if not st["hi_open"]:
            st["hi_open"] = True
            st["free"].extend(range(NSLOT_LO, 12))
            pump()

    load_const(cvec, cvec_d)
    load_const(bmod, bmod_d)
    load_const(n1g, n1g_d)
    load_const(n2g, n2g_d)
    load_const(identf, ident_d)
    load_const(sgug, sgug_d)
    load_const(convw, convw_d)
    load_const(convb, convb_d)
    load_const(gbv, gb_d)
    load_const(lam, lam_d)
    load_const(wrf, wr_d)
    load_const_cast(identb, ident_d)
    load_const_cast(wsTb, wsT_d)
    load_const_cast(wgb, wg_d)
    load_const_cast(sgubb, sgub_d)
    pump()

    if stop == "c0":
        return finish()
    P.add("pool", lambda e: e.memset(neghalf[:], -0.5), [], [neghalf.r()])

    def rstd_op(dst, src, lo, hi, inv_n):
        w = hi - lo
        P.add("pool", lambda e: e.tensor_scalar(out=dst[:].rearrange("p ... -> p (...)")[:, lo:hi] if False else _flat(dst)[:, lo:hi],
                                                in0=_flat(src)[:, lo:hi], scalar1=inv_n, scalar2=EPS,
                                                op0=ALU.mult, op1=ALU.add), [src.r(lo, hi)], [dst.r(lo, hi)])
        P.add("pool", lambda e: e.tensor_tensor(out=_flat(dst)[:, lo:hi], in0=_flat(dst)[:, lo:hi],
                                                in1=neghalf[:, 0:w], op=ALU.pow),
              [dst.r(lo, hi), neghalf.r()], [dst.r(lo, hi)])

    def _flat(buf):
        if len(buf.shape) == 2:
            return buf[:]
        return buf[:].rearrange("p a b -> p (a b)")

    P.add("pool", lambda e: e.memset(onesb[:], 1.0), [], [onesb.r()])
    P.add("pool", lambda e: e.memset(onesf[:], 1.0), [], [onesf.r()])
    P.add("act", lambda e: e.activation(out=scb[:], in_=cvec[:], func=AF.Silu), [cvec.r()], [scb.r()])
    P.add("act", lambda e: e.activation(out=cl[:], in_=lam[:], func=AF.Exp, scale=-1.0), [lam.r()], [cl.r()])
    P.add("act", lambda e: e.activation(out=cl[:], in_=cl[:], func=AF.Ln, bias=1.0, scale=1.0), [cl.r()], [cl.r()])
    P.add("dve", lambda e: e.tensor_scalar(out=cl2[:], in0=cl[:], scalar1=-16.0, scalar2=None, op0=ALU.mult),
          [cl.r()], [cl2.r()])
    P.add("dve", lambda e: e.tensor_scalar(out=cl[:], in0=cl[:], scalar1=-8.0, scalar2=None, op0=ALU.mult),
          [cl.r()], [cl.r()])

    if stop == "c1":
        return finish()
    modps = pmisc

    def mod_unit(j):
        s, buf = acquire()
        for mm in range(4):
            m = j * 4 + mm
            for k in range(8):
                P.add("pe", lambda e, buf=buf, mm=mm, m=m, k=k: e.matmul(
                    pmisc[:, 2 * m:2 * m + 2], lhsT=buf[:, k, mm * 128:(mm + 1) * 128], rhs=scb[:, k, :],
                    start=(k == 0), stop=(k == 7)),
                    [buf.r3(k, mm * 128, (mm + 1) * 128), scb.r()], [pmisc.r(2 * m, 2 * m + 2)])
        m0 = j * 4
        if stop == "m0":
            return
        P.add("dve", lambda e, m0=m0: e.tensor_tensor(
            out=modsb[:, m0:m0 + 4, :], in0=pmisc[:, 2 * m0:2 * m0 + 8].rearrange("p (m t) -> p m t", t=2),
            in1=bmod[:, m0:m0 + 4].unsqueeze(2).broadcast_to([128, 4, 2]), op=ALU.add),
            [pmisc.r(2 * m0, 2 * m0 + 8), bmod.r()], [modsb.r(2 * m0, 2 * m0 + 8)])
        release(s)

    for j in range(4):
        mod_unit(j)
    if stop in ("m0", "m1"):
        return finish()

    def mod_col(m0, which):
        return modsb[:, m0:m0 + 8, which]

    def mod1(m, which):
        return modsb[:, m, which:which + 1]

    P.add("dve", lambda e: e.scalar_tensor_tensor(out=gsc1x[:], in0=mod_col(8, 0), scalar=1.0, in1=n1g[:],
                                                  op0=ALU.add, op1=ALU.mult), [modsb.r(), n1g.r()], [gsc1x.r()])
    P.add("dve", lambda e: e.scalar_tensor_tensor(out=gsc1c[:], in0=mod_col(8, 1), scalar=1.0, in1=n1g[:],
                                                  op0=ALU.add, op1=ALU.mult), [modsb.r(), n1g.r()], [gsc1c.r()])

    if stop == "prologue":
        return finish()
    def phase_a_tile(src_rows, col, dstT, c0, gsc, which, par):
        xtb, xn = xt[par], xnb[par]
        dma("sp", f"xt{par}", xtb[:], src_rows, [], [xtb.r()])
        P.add("act", lambda e: e.activation(out=xn[:], in_=xtb[:], func=AF.Square, accum_out=ss[:, col:col + 1]),
              [xtb.r()], [xn.r(), ss.r(col, col + 1)])
        if stop == "a0":
            return
        rstd_op(rstd, ss, col, col + 1, 1.0 / D)
        if stop == "a1":
            return
        P.add("act", lambda e: e.activation(out=xn[:], in_=xtb[:], func=AF.Copy, scale=rstd[:, col:col + 1]),
              [xtb.r(), rstd.r(col, col + 1)], [xn.r()])
        if stop == "a2":
            return
        tb_ = tbanks[col % len(tbanks)]
        tv = tb_.bf
        for k in range(8):
            P.add("pe", lambda e, k=k: e.transpose(out=tv[:, k * 128:(k + 1) * 128],
                                                   in_=xn[:, k * 128:(k + 1) * 128], identity=identb[:]),
                  [xn.r(k * 128, (k + 1) * 128), identb.r()], [tb_.r()])
        if stop == "a3":
            return
        for k in range(8):
            if col % 2 == 0 or EVAC_ACT_ONLY:
                P.add("act", lambda e, k=k: e.activation(out=dstT[:, k, c0:c0 + 128],
                                                         in_=tv[:, k * 128:(k + 1) * 128],
                                                         func=AF.Identity, scale=gsc[:, k:k + 1],
                                                         bias=mod1(k, which)),
                      [tb_.r(), gsc.r(), modsb.r()], [dstT.r3(k, c0, c0 + 128)])
            else:
                P.add("dve", lambda e, k=k: e.tensor_scalar(out=dstT[:, k, c0:c0 + 128],
                                                            in0=tv[:, k * 128:(k + 1) * 128],
                                                            scalar1=gsc[:, k:k + 1], scalar2=mod1(k, which),
                                                            op0=ALU.mult, op1=ALU.add),
                      [tb_.r(), gsc.r(), modsb.r()], [dstT.r3(k, c0, c0 + 128)])

    tcount = 0
    for i in range(2):
        phase_a_tile(ctx_d[i * 128:(i + 1) * 128, :], i, hcT, i * 128, gsc1c, 1, tcount % 2)
        tcount += 1
        if stop in ("a0", "a1", "a2", "a3", "a4"):
            return finish()
    for i in range(16):
        phase_a_tile(x_d[i * 128:(i + 1) * 128, :], 2 + i, hxT, i * 128, gsc1x, 0, tcount % 2)
        tcount += 1

    if stop == "A":
        return finish()
    bank_rr = [0]

    def next_bank():
        b = pbank[bank_rr[0] % 4]
        bank_rr[0] += 1
        return b

    def proj_feature_major(buf, m, src, tb0, ncols, pb):
        for k in range(8):
            P.add("pe", lambda e, k=k: e.matmul(pb[:, 0:ncols], lhsT=buf[:, k, m * 128:(m + 1) * 128],
                                                rhs=src[:, k, tb0:tb0 + ncols], start=(k == 0), stop=(k == 7)),
                  [buf.r3(k, m * 128, (m + 1) * 128), src.r3(k, tb0, tb0 + ncols)], [pb.r(0, ncols)])

    s_u, bu = acquire()
    for m in range(4):
        for tb in range(4):
            pb = next_bank()
            proj_feature_major(bu, m, hxT, tb * 512, 512, pb)
            P.add("act", lambda e, m=m, tb=tb, pb=pb: e.activation(out=mixT[:, m, tb * 512:(tb + 1) * 512],
                                                                    in_=pb[:, :], func=AF.Gelu_apprx_tanh),
                  [pb.r()], [mixT.r3(m, tb * 512, (tb + 1) * 512)])
    release(s_u)

    if stop == "u":
        return finish()
    s_v, bv = acquire()
    sps = [pbank[4], pbank[5]]
    def v_stage1(i):
        pb = next_bank()
        for k in range(8):
            P.add("pe", lambda e, k=k, i=i, pb=pb: e.matmul(pb[:, :], lhsT=hxT[:, k, i * 128:(i + 1) * 128],
                                                            rhs=bv[:, k, :], start=(k == 0), stop=(k == 7)),
                  [hxT.r3(k, i * 128, (i + 1) * 128), bv.r3(k, 0, 512)], [pb.r()])
        gv = gvs[i % 4]
        P.add("act", lambda e, pb=pb, gv=gv: e.activation(out=gv[:], in_=pb[:, :], func=AF.Gelu_apprx_tanh),
              [pb.r()], [gv.r()])
        P.add("act", lambda e, gv=gv, i=i: e.activation(out=gsqfs[i % 2][:], in_=gv[:], func=AF.Square), [gv.r()],
              [gsqfs[i % 2].r()])

    def v_stage1b(i):
        P.add("dve", lambda e, i=i: e.tensor_reduce(out=ss4a[:, i, :], in_=gsqfs[i % 2][:].rearrange("p (h c) -> p h c", h=4),
                                                    axis=AX.X, op=ALU.add), [gsqfs[i % 2].r()], [ss4a.r3(i, 0, 4)])
        rstd_op(r4a, ss4a, i * 4, i * 4 + 4, 1.0 / 128)

    def v_stage2(i):
        gv = gvs[i % 4]
        vnb = vn[i % 2]
        for h in range(4):
            P.add("dve", lambda e, h=h, i=i, vnb=vnb, gv=gv: e.scalar_tensor_tensor(
                out=vnb[:, h * 128:(h + 1) * 128], in0=gv[:, h * 128:(h + 1) * 128], scalar=r4a[:, i, h:h + 1],
                in1=sgug[:, h * 128:(h + 1) * 128], op0=ALU.mult, op1=ALU.mult),
                [gv.r(h * 128, (h + 1) * 128), r4a.r3(i, 0, 4), sgug.r()], [vnb.r(h * 128, (h + 1) * 128)])

    def v_stage3(i):
        vnb = vn[i % 2]
        sp_ = sps[i % 2]
        for h in range(4):
            P.add("pe", lambda e, h=h, vnb=vnb, sp_=sp_: e.matmul(sp_[:, h * 128:(h + 1) * 128],
                                                                  lhsT=vnb[:, h * 128:(h + 1) * 128],
                                                                  rhs=wsTb[:, h, :], start=True, stop=False),
                  [vnb.r(h * 128, (h + 1) * 128), wsTb.r()], [sp_.r(h * 128, (h + 1) * 128)])
            P.add("pe", lambda e, h=h, sp_=sp_: e.matmul(sp_[:, h * 128:(h + 1) * 128], lhsT=onesb[0:1, :],
                                                         rhs=sgubb[0:1, h * 128:(h + 1) * 128],
                                                         start=False, stop=True),
                  [onesb.r(), sgubb.r()], [sp_.r(h * 128, (h + 1) * 128)])
        P.add("dve", lambda e, i=i, sp_=sp_: e.tensor_tensor(
            out=mixT[:, 0:4, i * 128:(i + 1) * 128], in0=mixT[:, 0:4, i * 128:(i + 1) * 128],
            in1=sp_[:, :].rearrange("p (h c) -> p h c", h=4), op=ALU.mult),
            [sp_.r()] + mixT.rks(range(4), i * 128, (i + 1) * 128), mixT.rks(range(4), i * 128, (i + 1) * 128))

    for t in range(16 + 3):
        if t < 16:
            v_stage1(t)
        if 0 <= t - 1 < 16:
            v_stage1b(t - 1)
        if 0 <= t - 2 < 16:
            v_stage2(t - 2)
        if 0 <= t - 3 < 16:
            v_stage3(t - 3)
    release(s_v)

    if stop == "v":
        return finish()
    s_g, bg = acquire()
    for m in range(4):
        for tb in range(4):
            pb = next_bank()
            proj_feature_major(bg, m, hxT, tb * 512, 512, pb)
            P.add("act", lambda e, m=m, tb=tb, pb=pb: e.activation(out=mixT[:, 4 + m, tb * 512:(tb + 1) * 512],
                                                                    in_=pb[:, :], func=AF.Gelu_apprx_tanh),
                  [pb.r()], [mixT.r3(4 + m, tb * 512, (tb + 1) * 512)])
    release(s_g)

    if stop == "g":
        return finish()
    s_x, bx = acquire()
    for cc in range(4):
        P.add("pool", lambda e: e.memset(xxp[:, :, 0:1], 0.0), [], [xxp.r()])
        P.add("pool", lambda e: e.memset(xxp[:, :, 65:67], 0.0), [], [xxp.r()])
        P.add("pool", lambda e: e.memset(xcp[:, 0:1], 0.0), [], [xcp.r(0, 1)])
        P.add("pool", lambda e: e.memset(xcp[:, 257:259], 0.0), [], [xcp.r(257, 259)])
        pb = next_bank()
        proj_feature_major(bx, cc, hcT, 0, CTX, pb)
        P.add("act", lambda e, pb=pb: e.activation(out=xcp[:, 1:257], in_=pb[:, 0:CTX], func=AF.Copy),
              [pb.r(0, CTX)], [xcp.r(1, 257)])
        for tb in range(4):
            pb = next_bank()
            proj_feature_major(bx, cc, hxT, tb * 512, 512, pb)
            P.add("act", lambda e, tb=tb, pb=pb: e.activation(
                out=xxp[:, tb * 8:(tb + 1) * 8, 1:65], in_=pb[:, :].rearrange("p (r c) -> p r c", c=64),
                func=AF.Copy), [pb.r()], [xxp.r(tb * 8 * 67, (tb + 1) * 8 * 67)])
        cx_lat = cx[:, 256:2304].rearrange("p (r c) -> p r c", c=64)
        P.add("act", lambda e, cc=cc: e.activation(out=cx_lat, in_=xxp[:, :, 0:64], func=AF.Identity,
                                                   scale=convw[:, cc, 0:1], bias=convb[:, cc:cc + 1]),
              [xxp.r(), convw.r(), convb.r()], [cx.r(256, 2304)])
        P.add("act", lambda e, cc=cc: e.activation(out=cx[:, 0:256], in_=xcp[:, 0:256], func=AF.Identity,
                                                   scale=convw[:, cc, 0:1], bias=convb[:, cc:cc + 1]),
              [xcp.r(), convw.r(), convb.r()], [cx.r(0, 256)])
        for k in range(1, 4):
            P.add("dve", lambda e, cc=cc, k=k: e.scalar_tensor_tensor(
                out=cx_lat, in0=xxp[:, :, k:k + 64], scalar=convw[:, cc, k:k + 1], in1=cx_lat,
                op0=ALU.mult, op1=ALU.add), [xxp.r(), convw.r(), cx.r(256, 2304)], [cx.r(256, 2304)])
            P.add("dve", lambda e, cc=cc, k=k: e.scalar_tensor_tensor(
                out=cx[:, 0:256], in0=xcp[:, k:k + 256], scalar=convw[:, cc, k:k + 1], in1=cx[:, 0:256],
                op0=ALU.mult, op1=ALU.add), [xcp.r(), convw.r(), cx.r(0, 256)], [cx.r(0, 256)])
        P.add("dve", lambda e: e.tensor_copy(out=cxb[:], in_=cx[:]), [cx.r()], [cxb.r()])
        for d in range(2):
            if d == 0:
                blocks = [(0, 256, 0)] + [(256 + t * 512, 512, 256 + t * 512) for t in range(4)]
            else:
                blocks = [(0, 256, 2048)] + [(256 + t * 512, 512, t * 512) for t in range(4)]
            ga = (d * 2 + 0) * 4 + cc
            gi = (d * 2 + 1) * 4 + cc
            for (s0, n, d0) in blocks:
                pb = next_bank()
                P.add("pe", lambda e, ga=ga, s0=s0, n=n, pb=pb: e.matmul(pb[:, 0:n], lhsT=wgb[:, ga, :],
                                                                         rhs=cxb[:, s0:s0 + n],
                                                                         start=True, stop=True),
                      [wgb.r(), cxb.r(s0, s0 + n)], [pb.r(0, n)])
                P.add("act", lambda e, ga=ga, n=n, d0=d0, pb=pb: e.activation(
                    out=Rb[:, d0:d0 + n], in_=pb[:, 0:n], func=AF.Sigmoid, bias=gbv[:, ga:ga + 1], scale=1.0),
                    [pb.r(0, n), gbv.r()], [Rb.r(d0, d0 + n)])
            for (s0, n, d0) in blocks:
                pb = next_bank()
                P.add("pe", lambda e, gi=gi, s0=s0, n=n, pb=pb: e.matmul(pb[:, 0:n], lhsT=wgb[:, gi, :],
                                                                         rhs=cxb[:, s0:s0 + n],
                                                                         start=True, stop=True),
                      [wgb.r(), cxb.r(s0, s0 + n)], [pb.r(0, n)])
                P.add("act", lambda e, gi=gi, n=n, d0=d0, pb=pb: e.activation(
                    out=Ib[:, d0:d0 + n], in_=pb[:, 0:n], func=AF.Sigmoid, bias=gbv[:, gi:gi + 1], scale=1.0),
                    [pb.r(0, n), gbv.r()], [Ib.r(d0, d0 + n)])
            ci = d * 4 + cc
            P.add("act", lambda e, ci=ci: e.activation(out=Sb[:], in_=Rb[:], func=AF.Exp,
                                                       scale=cl2[:, ci:ci + 1]), [Rb.r(), cl2.r()], [Sb.r()])
            P.add("act", lambda e, ci=ci: e.activation(out=Rb[:], in_=Rb[:], func=AF.Exp,
                                                       scale=cl[:, ci:ci + 1]), [Rb.r(), cl.r()], [Rb.r()])
            P.add("act", lambda e: e.activation(out=Sb[:], in_=Sb[:], func=AF.Sqrt, bias=1.0, scale=-1.0),
                  [Sb.r()], [Sb.r()])
            P.add("dve", lambda e: e.tensor_tensor(out=Sb[:], in0=Sb[:], in1=Ib[:], op=ALU.mult),
                  [Sb.r(), Ib.r()], [Sb.r()])
            if d == 0:
                P.add("dve", lambda e: e.tensor_tensor(out=Sb[:], in0=Sb[:], in1=cx[:], op=ALU.mult),
                      [Sb.r(), cx.r()], [Sb.r()])
                P.add("dve", lambda e: e.tensor_tensor_scan(out=Hf[:], data0=Rb[:], data1=Sb[:], initial=0.0,
                                                            op0=ALU.mult, op1=ALU.add),
                      [Rb.r(), Sb.r()], [Hf.r()])
            else:
                P.add("dve", lambda e: e.tensor_tensor(out=Sb[:, 0:2048], in0=Sb[:, 0:2048], in1=cx[:, 256:2304],
                                                       op=ALU.mult), [Sb.r(0, 2048), cx.r(256, 2304)],
                      [Sb.r(0, 2048)])
                P.add("dve", lambda e: e.tensor_tensor(out=Sb[:, 2048:2304], in0=Sb[:, 2048:2304],
                                                       in1=cx[:, 0:256], op=ALU.mult),
                      [Sb.r(2048, 2304), cx.r(0, 256)], [Sb.r(2048, 2304)])
                P.add("dve", lambda e: e.tensor_tensor_scan(out=Ib[:, ::-1], data0=Rb[:, ::-1], data1=Sb[:, ::-1],
                                                            initial=0.0, op0=ALU.mult, op1=ALU.add),
                      [Rb.r(), Sb.r()], [Ib.r()])
        P.add("dve", lambda e: e.tensor_tensor(out=Hf[:, 256:2304], in0=Hf[:, 256:2304], in1=Ib[:, 0:2048],
                                               op=ALU.add), [Hf.r(256, 2304), Ib.r(0, 2048)], [Hf.r(256, 2304)])
        P.add("dve", lambda e, cc=cc: e.tensor_tensor(out=mixT[:, 4 + cc, :], in0=mixT[:, 4 + cc, :],
                                                      in1=Hf[:, 256:2304], op=ALU.mult),
              [mixT.r3(4 + cc, 0, S), Hf.r(256, 2304)], [mixT.r3(4 + cc, 0, S)])
    release(s_x)
    st["free"].extend([12, 13])
    pump()
    if debug:
        dma("sp", "dbg_mixT", dbg["mixT"], mixT[:], [mixT.r()], [])

    if stop == "RG":
        return finish()
    for j in range(4, 12):
        mod_unit(j)
    if debug:
        dma("sp", "dbg_mod", dbg["mod"], modsb[:], [modsb.r()], [])
    P.add("dve", lambda e: e.scalar_tensor_tensor(out=gsc2x[:], in0=mod_col(32, 0), scalar=1.0, in1=n2g[:],
                                                  op0=ALU.add, op1=ALU.mult), [modsb.r(), n2g.r()], [gsc2x.r()])

    def build_rep(rep, m0, dgb):
        for k in range(8):
            P.add("dve", lambda e, k=k: e.tensor_scalar(out=dgb[:], in0=identf[:], scalar1=mod1(m0 + k, 0),
                                                        scalar2=None, op0=ALU.mult),
                  [identf.r(), modsb.r()], [dgb.r()])
            pb = next_bank()
            P.add("pe", lambda e, pb=pb: e.matmul(pb[:, 0:128], lhsT=onesf[:], rhs=dgb[:], start=True, stop=True),
                  [onesf.r(), dgb.r()], [pb.r(0, 128)])
            P.add("act", lambda e, k=k, pb=pb: e.activation(out=rep[:, k * 128:(k + 1) * 128], in_=pb[:, 0:128],
                                                            func=AF.Copy), [pb.r(0, 128)],
                  [rep.r(k * 128, (k + 1) * 128)])

    build_rep(g1x_rep, 16, dg)
    wo = []
    for n in range(2):
        s, b = acquire()
        wo.append((s, b))
        eng = "dve" if n == 0 else "pool"
        for k in range(8):
            P.add(eng, lambda e, b=b, k=k, n=n: e.tensor_tensor(out=b[:, k, :], in0=b[:, k, :],
                                                                in1=g1x_rep[:, n * 512:(n + 1) * 512],
                                                                op=ALU.mult),
                  [b.r3(k, 0, 512), g1x_rep.r(n * 512, (n + 1) * 512)], [b.r3(k, 0, 512)])

    load_c2 = lambda buf, src: dma("sp", "T:const2", buf[:], src, [], [buf.r()])
    load_c2(fg_rep, fg_d)
    load_c2(iota256, iota_d)
    load_c2(blkf, blk_d)
    load_c2(selfull, self_d)
    dma("pool", "T:constp2", ltrib[:], ltri_d, [], [ltrib.r()])
    dma("pool", "T:constp2", tinfob[:], tinfo_d, [], [tinfob.r()])
    build_rep(g2x_rep, 40, dg2)
    P.add("dve", lambda e: e.tensor_tensor(out=wrp[:], in0=wrf[:],
                                           in1=gsc2x[:].unsqueeze(2).broadcast_to([128, 8, 16]), op=ALU.mult),
          [wrf.r(), gsc2x.r()], [wrp.r()])
    P.add("dve", lambda e: e.tensor_copy(out=wrb[:], in_=wrf[:]), [wrf.r()], [wrb.r()])
    P.add("dve", lambda e: e.tensor_copy(out=sh2b[:], in_=mod_col(24, 0)), [modsb.r()], [sh2b.r()])
    for k in range(8):
        P.add("pe", lambda e, k=k: e.matmul(pmisc[0:1, 0:16], lhsT=sh2b[:, k:k + 1], rhs=wrb[:, k, :],
                                            start=(k == 0), stop=(k == 7)),
              [sh2b.r(), wrb.r()], [pmisc.r(0, 16)])
    P.add("dve", lambda e: e.tensor_copy(out=rbiasb[:], in_=pmisc[0:1, 0:16]), [pmisc.r(0, 16)], [rbiasb.r()])

    if stop == "prep":
        return finish()
    lgb = [pmisc, Buf(trps.t[:].bitcast(F32), "ps", trps.off, [128, 512], F32)]
    lgb[1].t = None
    lgv = [pmisc[:, 0:16], trps.t[:].bitcast(F32).rearrange("p a b -> p (a b)")[:, 0:16]]
    trb = [pbank[4], pbank[5]]
    def cd_vars(i):
        par = i % 2
        return par, x1t[par], xn2b[par], xn2Ts[par], lgv[par], lgb[par], exls[par], trb[par]

    def cd_stage1(i):
        par, x1, xn2, xT, lg, lgB, exl_, tb_ = cd_vars(i)
        if i == 0:
            dma("sp", "x1t0", x1t[0][:], x_d[0:128, :], [], [x1t[0].r()])
        if i + 1 < 16:
            j = i + 1
            dma("sp", f"x1t{j % 2}", x1t[j % 2][:], x_d[j * 128:(j + 1) * 128, :], [], [x1t[j % 2].r()])
        for n in range(2):
            pb = next_bank()
            wb_ = wo[n][1]
            for k in range(8):
                P.add("pe", lambda e, k=k, i=i, pb=pb, wb_=wb_: e.matmul(
                    pb[:, :], lhsT=mixT[:, k, i * 128:(i + 1) * 128], rhs=wb_[:, k, :],
                    start=(k == 0), stop=(k == 7)),
                    [mixT.r3(k, i * 128, (i + 1) * 128), wb_.r3(k, 0, 512)], [pb.r()])
            P.add("dve", lambda e, n=n, pb=pb, x1=x1: e.tensor_tensor(out=x1[:, n * 512:(n + 1) * 512],
                                                                      in0=x1[:, n * 512:(n + 1) * 512],
                                                                      in1=pb[:, :], op=ALU.add),
                  [x1.r(n * 512, (n + 1) * 512), pb.r()], [x1.r(n * 512, (n + 1) * 512)])
        dma("sp", f"accw{par}", acc_dram[i * 128:(i + 1) * 128, :], x1[:], [x1.r()], [("dram", "acc", i)])
        if debug:
            dma("sp", f"dbg_x1{par}", dbg["x1"][i * 128:(i + 1) * 128, :], x1[:], [x1.r()], [])
        P.add("act", lambda e, i=i, x1=x1, xn2=xn2: e.activation(out=xn2[:, 0:D], in_=x1[:], func=AF.Square,
                                                                 accum_out=ss2[:, i:i + 1]),
              [x1.r()], [xn2.r(0, D), ss2.r(i, i + 1)])
        rstd_op(rstd2, ss2, i, i + 1, 1.0 / D)
        P.add("act", lambda e, i=i, x1=x1, xn2=xn2: e.activation(out=xn2[:, 0:D], in_=x1[:], func=AF.Copy,
                                                                 scale=rstd2[:, i:i + 1]),
              [x1.r(), rstd2.r(i, i + 1)], [xn2.r(0, D)])
        dma("sp", f"xn2w{par}", xn2_dram[i * 128:(i + 1) * 128, 0:D], xn2[:, 0:D], [xn2.r(0, D)],
            [("dram", "xn2", i)])

    def cd_stage2(i):
        par, x1, xn2, xT, lg, lgB, exl_, tb_ = cd_vars(i)
        for k in range(8):
            P.add("pe", lambda e, k=k, xn2=xn2, tb_=tb_: e.transpose(
                out=tb_.bf[:, k * 128:(k + 1) * 128], in_=xn2[:, k * 128:(k + 1) * 128], identity=identb[:]),
                [xn2.r(k * 128, (k + 1) * 128), identb.r()], [tb_.r()])
        P.add("act", lambda e, tb_=tb_, xT=xT: e.activation(out=xT[:].rearrange("p a b -> p (a b)"),
                                                            in_=tb_.bf[:, 0:D], func=AF.Copy),
              [tb_.r()], [xT.r()])

    def cd_stage3(i):
        par, x1, xn2, xT, lg, lgB, exl_, tb_ = cd_vars(i)
        for k in range(8):
            P.add("pe", lambda e, k=k, xT=xT, lg=lg: e.matmul(lg, lhsT=xT[:, k, :], rhs=wrp[:, k, :],
                                                              start=(k == 0), stop=False),
                  [xT.r3(k, 0, 128), wrp.r()], [lgB.r(0, 16)])
        P.add("pe", lambda e, lg=lg: e.matmul(lg, lhsT=onesb[0:1, :], rhs=rbiasb[0:1, :],
                                              start=False, stop=True), [onesb.r(), rbiasb.r()], [lgB.r(0, 16)])
        P.add("dve", lambda e, i=i, lg=lg: e.tensor_reduce(out=smxa[:, i, 0:1], in_=lg, axis=AX.X, op=ALU.max),
              [lgB.r(0, 16)], [smxa.r3(i, 0, 1)])
        P.add("dve", lambda e, i=i: e.tensor_scalar(out=smxa[:, i, 1:2], in0=smxa[:, i, 0:1], scalar1=-1.0,
                                                    scalar2=None, op0=ALU.mult), [smxa.r3(i, 0, 1)],
              [smxa.r3(i, 1, 2)])
        P.add("act", lambda e, i=i, lg=lg, exl_=exl_: e.activation(out=exl_[:], in_=lg, func=AF.Exp,
                                                                   bias=smxa[:, i, 1:2], scale=1.0,
                                                                   accum_out=smxa[:, i, 2:3]),
              [lgB.r(0, 16), smxa.r3(i, 1, 2)], [exl_.r(), smxa.r3(i, 2, 3)])
        P.add("dve", lambda e, i=i: e.reciprocal(out=smxa[:, i, 3:4], in_=smxa[:, i, 2:3]), [smxa.r3(i, 2, 3)],
              [smxa.r3(i, 3, 4)])
        P.add("dve", lambda e, i=i, exl_=exl_: e.tensor_scalar(out=aff_all[:, i, :], in0=exl_[:],
                                                               scalar1=smxa[:, i, 3:4], scalar2=None,
                                                               op0=ALU.mult),
              [exl_.r(), smxa.r3(i, 3, 4)], [aff_all.r3(i, 0, 16)])

    for t in range(16 + 2):
        if t < 16:
            cd_stage1(t)
        if 0 <= t - 1 < 16:
            cd_stage2(t - 1)
        if 0 <= t - 2 < 16:
            cd_stage3(t - 2)
    aff_flat0 = aff_all[:].rearrange("p i e -> p (i e)")
    tl3 = tail_all[:].rearrange("p i (e t) -> p (i e) t", t=3)
    P.add("dve", lambda e: e.tensor_copy(out=tl3[:, :, 0], in_=aff_flat0), [aff_all.r()], [tail_all.r()])
    P.add("dve", lambda e: e.tensor_tensor(out=rk[:], in0=aff_flat0, in1=tl3[:, :, 0], op=ALU.subtract),
          [aff_all.r(), tail_all.r()], [rk.r()])
    P.add("dve", lambda e: e.tensor_copy(out=tl3[:, :, 1], in_=rk[:]), [rk.r()], [tail_all.r()])
    P.add("dve", lambda e: e.tensor_tensor(out=rk[:], in0=rk[:], in1=tl3[:, :, 1], op=ALU.subtract),
          [rk.r(), tail_all.r()], [rk.r()])
    P.add("dve", lambda e: e.tensor_copy(out=tl3[:, :, 2], in_=rk[:]), [rk.r()], [tail_all.r()])
    dma("sp", "tailw", xn2_dram.rearrange("(i p) c -> p i c", p=128)[:, :, D:XROW], tail_all[:],
        [tail_all.r()], [("dram", "xn2", i) for i in range(16)])
    release(wo[0][0])
    release(wo[1][0])
    open_hi_slots()
    if debug:
        dma("sp", "dbg_aff", dbg["aff"], aff_all[:], [aff_all.r()], [])

    if stop == "D":
        return finish()
    for h in range(2):
        P.add("dve", lambda e, h=h: e.tensor_copy(
            out=affg[:, h, :].rearrange("p (e s) -> p e s", s=8),
            in_=aff_all[:, h::2, :].rearrange("p s e -> p e s")), [aff_all.r()], [affg.r3(h, 0, 128)])
        P.add("pe", lambda e, h=h: e.transpose(out=pmisc[:, 128 + h * 128:256 + h * 128], in_=affg[:, h, :],
                                               identity=identf[:]),
              [affg.r3(h, 0, 128), identf.r()], [pmisc.r(128 + h * 128, 256 + h * 128)])
    P.add("dve", lambda e: e.tensor_copy(out=affT[:], in_=pmisc[:, 128:384]), [pmisc.r(128, 384)], [affT.r()])
    P.add("dve", lambda e: e.memset(lo_t[:], 0.0), [], [lo_t.r()])
    P.add("dve", lambda e: e.memset(cnt2[:], 0.0), [], [cnt2.r()])
    for it in range(NBISECT):
        hk = 2.0 ** (-(it + 1))
        P.add("dve", lambda e, hk=hk: e.tensor_scalar(out=mid_t[:], in0=lo_t[:], scalar1=hk, scalar2=None,
                                                      op0=ALU.add), [lo_t.r()], [mid_t.r()])
        P.add("dve", lambda e: e.tensor_scalar(out=cmpj[:], in0=affT[:], scalar1=mid_t[:, 0:1], scalar2=None,
                                               op0=ALU.is_ge, op1=ALU.add, accum_out=cnt2[:, 0:1]),
              [affT.r(), mid_t.r()], [cmpj.r(), cnt2.r(0, 1)])
        P.add("pe", lambda e: e.matmul(pmisc[:, 0:2], lhsT=blkf[:], rhs=cnt2[:], start=True, stop=True),
              [blkf.r(), cnt2.r()], [pmisc.r(0, 2)])
        P.add("dve", lambda e, hk=hk: e.tensor_scalar(out=stp[:], in0=pmisc[:, 0:1], scalar1=CAP - 0.5,
                                                      scalar2=hk, op0=ALU.is_ge, op1=ALU.mult),
              [pmisc.r(0, 1)], [stp.r()])
        P.add("dve", lambda e: e.tensor_tensor(out=lo_t[:], in0=lo_t[:], in1=stp[:], op=ALU.add),
              [lo_t.r(), stp.r()], [lo_t.r()])
    P.add("dve", lambda e: e.tensor_scalar(out=selt[:], in0=selfull[:], scalar1=lo_t[:, 0:1], scalar2=None,
                                           op0=ALU.mult), [selfull.r(), lo_t.r()], [selt.r()])
    thr_ps = pbank[0]
    P.add("pe", lambda e: e.matmul(thr_ps[:, 0:256], lhsT=onesf[:], rhs=selt[:], start=True, stop=True),
          [onesf.r(), selt.r()], [thr_ps.r(0, 256)])
    aff_flat = aff_all[:].rearrange("p i e -> p (i e)")
    P.add("dve", lambda e: e.tensor_tensor(out=maskf[:], in0=aff_flat, in1=thr_ps[:, 0:256], op=ALU.is_ge),
          [aff_all.r(), thr_ps.r(0, 256)], [maskf.r()])
    P.add("dve", lambda e: e.tensor_copy(out=maskb[:], in_=maskf[:]), [maskf.r()], [maskb.r()])
    tot_ps = pbank[1]
    rank_ps = pbank[2]
    P.add("pe", lambda e: e.matmul(tot_ps[:, 0:256], lhsT=onesb[:], rhs=maskb[:], start=True, stop=True),
          [onesb.r(), maskb.r()], [tot_ps.r(0, 256)])
    P.add("pe", lambda e: e.matmul(rank_ps[:, 0:256], lhsT=ltrib[:], rhs=maskb[:], start=True, stop=True),
          [ltrib.r(), maskb.r()], [rank_ps.r(0, 256)])
    P.add("dve", lambda e: e.memset(offt[:, 0, :], 0.0), [], [offt.r3(0, 0, 16)])
    for i in range(1, 16):
        P.add("dve", lambda e, i=i: e.tensor_tensor(out=offt[:, i, :], in0=offt[:, i - 1, :],
                                                    in1=tot_ps[:, (i - 1) * 16:i * 16], op=ALU.add),
              [offt.r3(i - 1, 0, 16), tot_ps.r((i - 1) * 16, i * 16)], [offt.r3(i, 0, 16)])
    P.add("dve", lambda e: e.tensor_tensor(out=rk[:], in0=offt[:].rearrange("p i e -> p (i e)"),
                                           in1=rank_ps[:, 0:256], op=ALU.add),
          [offt.r(), rank_ps.r(0, 256)], [rk.r()])
    P.add("dve", lambda e: e.scalar_tensor_tensor(out=rankm[:], in0=rk[:], scalar=1.0, in1=maskf[:],
                                                  op0=ALU.add, op1=ALU.mult), [rk.r(), maskf.r()], [rankm.r()])
    P.add("dve", lambda e: e.tensor_scalar(out=rankm[:], in0=rankm[:], scalar1=-1.0, scalar2=None, op0=ALU.add),
          [rankm.r()], [rankm.r()])
    if debug:
        dma("sp", "dbg_rankm", dbg["rankm"], rankm[:], [rankm.r()], [])

    if stop == "E":
        return finish()
    xn2_keys = [("dram", "xn2", i) for i in range(16)]
    acc_keys = [("dram", "acc", i) for i in range(16)]

    def s_build(e_):
        for i in range(16):
            P.add("dve", lambda e, i=i, e_=e_: e.tensor_scalar(out=Se[:, i, :], in0=iota256[:],
                                                               scalar1=rankm[:, i * 16 + e_:i * 16 + e_ + 1],
                                                               scalar2=None, op0=ALU.is_equal),
                  [iota256.r(), rankm.r(i * 16 + e_, i * 16 + e_ + 1)], [Se.r3(i, 0, 256)])

    def idx_gather(e_):
        for half in range(2):
            for i in range(16):
                P.add("pe", lambda e, i=i, half=half: e.matmul(pmisc[:, 32 + 2 * half:34 + 2 * half],
                                                               lhsT=Se[:, i, half * 128:(half + 1) * 128],
                                                               rhs=tinfob[:, i, :], start=(i == 0), stop=(i == 15)),
                      [Se.r3(i, half * 128, (half + 1) * 128), tinfob.r()],
                      [pmisc.r(32 + 2 * half, 34 + 2 * half)])
        P.add("dve", lambda e, e_=e_: e.tensor_reduce(out=idxf[:],
                                                      in_=pmisc[:, 32:36].rearrange("p (h t) -> p h t", t=2),
                                                      axis=AX.X, op=ALU.add),
              [pmisc.r(32, 36)], [idxf.r()])
        P.add("dve", lambda e, e_=e_: e.tensor_copy(out=idx_all[:, e_, :], in_=idxf[:]),
              [idxf.r()], [idx_all.r3(e_, 0, 2)])
        for half in range(2):
            P.add("pool", lambda e, half=half, e_=e_: e.indirect_dma_start(
                out=xg[:, half, :], out_offset=None, in_=xn2_dram[:, :],
                in_offset=bass.IndirectOffsetOnAxis(ap=idx_all[:, e_, half:half + 1], axis=0)),
                [idx_all.r3(e_, half, half + 1)] + xn2_keys, [xg.r3(half, 0, XROW)], dma=f"xg{half}")

    def xe_transpose(e_):
        for half in range(2):
            for k in range(8):
                P.add("pe", lambda e, k=k, half=half: e.transpose(
                    out=trps[:, k, :], in_=xg[:, half, k * 128:(k + 1) * 128], identity=identb[:]),
                    [xg.r3(half, k * 128, (k + 1) * 128), identb.r()], [trps.r3(k, 0, 128)])
            for k in range(8):
                if half == 0:
                    P.add("act", lambda e, k=k, half=half: e.activation(
                        out=xeT[:, k, half * 128:(half + 1) * 128], in_=trps[:, k, :], func=AF.Identity,
                        scale=gsc2x[:, k:k + 1], bias=mod1(24 + k, 0)),
                        [trps.r3(k, 0, 128), gsc2x.r(), modsb.r()], [xeT.r3(k, half * 128, (half + 1) * 128)])
                else:
                    P.add("dve", lambda e, k=k, half=half: e.tensor_scalar(
                        out=xeT[:, k, half * 128:(half + 1) * 128], in0=trps[:, k, :], scalar1=gsc2x[:, k:k + 1],
                        scalar2=mod1(24 + k, 0), op0=ALU.mult, op1=ALU.add),
                        [trps.r3(k, 0, 128), gsc2x.r(), modsb.r()], [xeT.r3(k, half * 128, (half + 1) * 128)])

    ps1q = (pbank[4], pbank[0])
    ps3q = (pbank[5], pbank[1])

    def expert_h13(e_):
        for cb in range(4):
            sa, b1 = acquire()
            sb_, b3 = acquire()
            for m in range(4):
                hm = cb * 4 + m
                q = m % 2
                ps1, ps3 = ps1q[q], ps3q[q]
                for (pb, bw) in ((ps1, b1), (ps3, b3)):
                    for k in range(8):
                        P.add("pe", lambda e, k=k, m=m, pb=pb, bw=bw: e.matmul(
                            pb[:, 0:256], lhsT=bw[:, k, m * 128:(m + 1) * 128], rhs=xeT[:, k, :],
                            start=(k == 0), stop=(k == 7)),
                            [bw.r3(k, m * 128, (m + 1) * 128), xeT.r3(k, 0, 256)],
                            [pb.r(0, 256)])
                slb = sl[q]
                P.add("act", lambda e, ps1=ps1, slb=slb: e.activation(out=slb[:], in_=ps1[:, 0:256],
                                                                      func=AF.Silu),
                      [ps1.r(0, 256)], [slb.r()])
                P.add("dve", lambda e, ps3=ps3, hm=hm, slb=slb: e.tensor_tensor(
                    out=hidT[:, hm, :], in0=slb[:], in1=ps3[:, 0:256], op=ALU.mult),
                    [slb.r(), ps3.r(0, 256)], [hidT.r3(hm, 0, 256)])
            release(sa)
            release(sb_)

    gate_all = mb("gate_all", [128, NE, 2], F32)

    def gate_copy(e_):
        for half in range(2):
            P.add("dve", lambda e, half=half, e_=e_: e.tensor_reduce(
                out=gate_all[:, e_, half:half + 1], in_=xg[:, half, D + 3 * e_:D + 3 * e_ + 3], axis=AX.X,
                op=ALU.add), [xg.r3(half, D, XROW)], [gate_all.r3(e_, half, half + 1)])

    def expert_w2_v2(e_):
        for kg in range(4):
            s2, b2 = acquire()
            for kk in range(4):
                k = kg * 4 + kk
                for half in range(2):
                    for n in range(2):
                        pb = pbank[half * 2 + n]
                        P.add("pe", lambda e, k=k, kk=kk, half=half, n=n, pb=pb, b2=b2: e.matmul(
                            pb[:, :], lhsT=hidT[:, k, half * 128:(half + 1) * 128],
                            rhs=b2[:, kk, n * 512:(n + 1) * 512], start=(k == 0), stop=(k == 15)),
                            [hidT.r3(k, half * 128, (half + 1) * 128), b2.r3(kk, n * 512, (n + 1) * 512)],
                            [pb.r()])
            release(s2)
        for half in range(2):
            for n in range(2):
                pb = pbank[half * 2 + n]
                P.add("dve", lambda e, half=half, n=n, pb=pb, e_=e_: e.scalar_tensor_tensor(
                    out=ye[:, half, n * 512:(n + 1) * 512], in0=pb[:, :], scalar=gate_all[:, e_, half:half + 1],
                    in1=g2x_rep[:, n * 512:(n + 1) * 512], op0=ALU.mult, op1=ALU.mult),
                    [pb.r(), gate_all.r3(e_, half, half + 1), g2x_rep.r(n * 512, (n + 1) * 512)],
                    [ye.r3(half, n * 512, (n + 1) * 512)])
        for half in range(2):
            P.add("pool", lambda e, half=half, e_=e_: e.indirect_dma_start(
                out=acc_dram[:, :], out_offset=bass.IndirectOffsetOnAxis(ap=idx_all[:, e_, half:half + 1], axis=0),
                in_=ye[:, half, :], in_offset=None, compute_op=ALU.add),
                [ye.r3(half, 0, D), idx_all.r3(e_, half, half + 1)] + acc_keys, acc_keys, dma=f"scat{half}")

    s_build(0)
    idx_gather(0)
    xe_transpose(0)
    gate_copy(0)
    s_build(1)
    idx_gather(1)
    for e_ in range(NE):
        expert_h13(e_)
        if e_ + 2 < NE:
            s_build(e_ + 2)
        if e_ + 1 < NE:
            xe_transpose(e_ + 1)
            gate_copy(e_ + 1)
        expert_w2_v2(e_)
        if e_ + 2 < NE:
            idx_gather(e_ + 2)

    if stop == "F":
        return finish()
    x2bufs = [x1t[0], x1t[1], x2e[0], x2e[1]]
    x2grp = ["x1t0", "x1t1", "x2e0", "x2e1"]
    for i in range(16):
        par = i % 2
        x2, ot = x2bufs[i % 4], outt[i % 4]
        junk = xn2b[par]
        if i == 0:
            for j in range(3):
                dma("sp", x2grp[j % 4], x2bufs[j % 4][:], acc_dram[j * 128:(j + 1) * 128, :], acc_keys,
                    [x2bufs[j % 4].r()])
        if i + 3 < 16:
            j = i + 3
            dma("sp", x2grp[j % 4], x2bufs[j % 4][:], acc_dram[j * 128:(j + 1) * 128, :], acc_keys,
                [x2bufs[j % 4].r()])
        P.add("act", lambda e, i=i, x2=x2, junk=junk: e.activation(out=junk[:, 0:D], in_=x2[:], func=AF.Square,
                                                                   accum_out=ss3[:, i:i + 1]),
              [x2.r()], [junk.r(0, D), ss3.r(i, i + 1)])
        rstd_op(rstd3, ss3, i, i + 1, 1.0 / D)
        if i % 2 == 0:
            P.add("dve", lambda e, i=i, x2=x2, ot=ot: e.scalar_tensor_tensor(
                out=ot[:], in0=x2[:], scalar=rstd3[:, i:i + 1], in1=fg_rep[:], op0=ALU.mult, op1=ALU.mult),
                [x2.r(), rstd3.r(i, i + 1), fg_rep.r()], [ot.r()])
        else:
            P.add("act", lambda e, i=i, x2=x2, ot=ot: e.activation(out=ot[:], in_=x2[:], func=AF.Copy,
                                                                   scale=rstd3[:, i:i + 1]),
                  [x2.r(), rstd3.r(i, i + 1)], [ot.r()])
            P.add("pool", lambda e, ot=ot: e.tensor_tensor(out=ot[:], in0=ot[:], in1=fg_rep[:], op=ALU.mult),
                  [ot.r(), fg_rep.r()], [ot.r()])
        dma("sp", f"outw{i % 4}", out_d[i * 128:(i + 1) * 128, :], ot[:], [ot.r()], [("dram", "out", i)])

    with ExitStack() as stack:
        P.emit(stack)
    return nc


_CACHE = {}


def _fm(v, k):
    return np.ascontiguousarray(np.asarray(v, np.float32).reshape(k, 128).T)


def make_in_maps(x, c, ctx, c_ctx, w_mod, b_mod, norm1_g, norm2_g, w_in, sgu_g, sgu_w, sgu_b, conv_w, conv_b,
                 rg_wa, rg_ba, rg_wi, rg_bi, rg_lam, w_out, w_router, w1, w3, w2, final_g):
    f = lambda a: np.ascontiguousarray(np.asarray(a, dtype=np.float32))
    x, c, ctx, c_ctx = f(x), f(c), f(ctx), f(c_ctx)
    shared = {}
    shared["w_mod"] = f(w_mod[0])
    shared["bmod"] = _fm(b_mod[0], 48)
    shared["n1g"] = _fm(norm1_g[0], 8)
    shared["n2g"] = _fm(norm2_g[0], 8)
    shared["w_in"] = f(w_in[0])
    shared["sgug_rep"] = np.ascontiguousarray(np.broadcast_to(f(sgu_g[0])[None, :], (128, 512)))
    shared["wsT"] = np.ascontiguousarray(np.transpose(f(sgu_w[0]), (2, 0, 1)))
    shared["sgub"] = f(sgu_b[0]).reshape(1, 512)
    cw = f(conv_w[0])
    shared["convw"] = np.ascontiguousarray(np.transpose(cw.reshape(4, 4, 128), (2, 1, 0)))
    shared["convb"] = _fm(conv_b[0], 4)
    wg = np.zeros((128, 16, 128), np.float32)
    gb = np.zeros((128, 16), np.float32)
    lam = np.zeros((128, 8), np.float32)
    wa, wi = f(rg_wa[0]), f(rg_wi[0])
    ba, bi, lm = f(rg_ba[0]), f(rg_bi[0]), f(rg_lam[0])
    for d in range(2):
        for cc in range(4):
            for g, (w_, b_) in enumerate(((wa, ba), (wi, bi))):
                idx = (d * 2 + g) * 4 + cc
                for hh in range(2):
                    wg[hh * 64:(hh + 1) * 64, idx, hh * 64:(hh + 1) * 64] = w_[d, 2 * cc + hh]
                gb[:, idx] = b_[d, cc * 128:(cc + 1) * 128]
            lam[:, d * 4 + cc] = lm[d, cc * 128:(cc + 1) * 128]
    shared["wg"], shared["gb"], shared["lam"] = wg, gb, lam
    shared["w_out"] = f(w_out[0])
    shared["wr"] = np.ascontiguousarray(f(w_router[0]).reshape(8, 128, 16).transpose(1, 0, 2))
    shared["w1"], shared["w3"], shared["w2"] = f(w1[0]), f(w3[0]), f(w2[0])
    shared["fg_rep"] = np.ascontiguousarray(np.broadcast_to(f(final_g)[None, :], (128, D)))
    shared["ident"] = np.eye(128, dtype=np.float32)
    shared["iota256"] = np.ascontiguousarray(np.broadcast_to(np.arange(256, dtype=np.float32)[None, :], (128, 256)))
    kk, mm = np.meshgrid(np.arange(128), np.arange(128), indexing="ij")
    shared["ltri"] = (kk < mm).astype(np.float32)
    shared["blk"] = ((kk // 8) == (mm // 8)).astype(np.float32)
    sel = np.zeros((128, 16, 16), np.float32)
    for e in range(16):
        sel[8 * e, :, e] = 1.0
    shared["selfull"] = sel.reshape(128, 256)
    ti = np.zeros((128, 16, 2), np.float32)
    ti[:, :, 0] = np.arange(128)[:, None]
    ti[:, :, 1] = (np.arange(16) * 128)[None, :]
    shared["tinfo"] = ti
    cctx_fm = _fm(c_ctx, 8)
    in_maps = []
    for b in range(NCORES):
        m = dict(shared)
        m["x"] = x[b]
        m["ctx"] = ctx[b]
        cv = np.zeros((128, 8, 2), np.float32)
        cv[:, :, 0] = _fm(c[b], 8)
        cv[:, :, 1] = cctx_fm
        m["cvec"] = cv
        in_maps.append(m)
    return in_maps


def kernel(**inputs):
    if "nc" not in _CACHE:
        _CACHE["nc"] = build_program()
    nc = _CACHE["nc"]
    in_maps = make_in_maps(**inputs)
    res = run_bass_kernel_spmd(nc, in_maps, core_ids=list(range(NCORES)))
    out = np.stack([np.asarray(r["out"], dtype=np.float32) for r in res.results], axis=0)
    return out
```

```python
import numpy as np
import concourse.bass as bass
import concourse.mybir as mybir
from concourse.bass_utils import run_bass_kernel_spmd

F32 = mybir.dt.float32
BF16 = mybir.dt.bfloat16
I32 = mybir.dt.int32
AF = mybir.ActivationFunctionType
ALU = mybir.AluOpType
AX = mybir.AxisListType

NCORES = 8
S = 2048
D = 1024
CTX = 256
NE = 16
CAP = 256
EPS = 1e-6
XROW = 1072
NSLOT_LO = 8
NSLOT = 14
SLOT_BYTES = 8192
NBISECT = 28
SAME_ENGINE_SYNC = True
import os as _os
EVAC_ACT_ONLY = _os.environ.get('EVAC_ACT_ONLY', '0') == '1'

_ESZ = {F32: 4, BF16: 2, I32: 4}


class Buf:
    def __init__(self, t, space, off, shape, dt):
        self.t, self.space, self.off, self.shape, self.dt = t, space, off, shape, dt
        self.esz = _ESZ[dt]
        n = 1
        for s in shape[1:]:
            n *= s
        self.nbytes = n * self.esz

    def __getitem__(self, k):
        return self.t[k]

    def r(self, lo=None, hi=None):
        lo = 0 if lo is None else lo
        hi = (self.nbytes // self.esz) if hi is None else hi
        return (self.space, self.off + lo * self.esz, self.off + hi * self.esz)

    def r3(self, k, c0, c1):
        n = self.shape[-1]
        return self.r(k * n + c0, k * n + c1)

    def rks(self, ks, c0, c1):
        return [self.r3(k, c0, c1) for k in ks]


class Op:
    __slots__ = ("eng", "fn", "deps", "is_dma", "sem", "val", "signal", "cnt", "grp")


class Prog:
    GRAN = 64

    def __init__(self, nc):
        self.nc = nc
        self.ops = []
        self.by_eng = {e: [] for e in ("pe", "act", "dve", "pool", "sp")}
        self.blocks = {}
        self.keys = {}
        self.dma_groups = {}

    def _state(self, key):
        st = self.blocks.get(key)
        if st is None:
            st = [None, {}, []]
            self.blocks[key] = st
        return st

    def _keys_of(self, rng):
        if rng[0] == "dram":
            return [rng]
        space, lo, hi = rng
        gran = 2048 if space == "ps" else self.GRAN
        return [(space, b) for b in range(lo // gran, (hi - 1) // gran + 1)]

    def add(self, eng, fn, reads=(), writes=(), dma=None):
        op = Op()
        op.eng, op.fn, op.is_dma, op.signal, op.cnt, op.grp = eng, fn, dma is not None, False, None, dma
        oid = len(self.ops)
        deps = set()
        rkeys = []
        wkeys = []
        for rng in reads:
            (wkeys if rng[0] == "ps" else rkeys).extend(self._keys_of(rng))
        for rng in writes:
            wkeys.extend(self._keys_of(rng))
        for k in rkeys:
            st = self._state(k)
            if st[0] is not None:
                deps.add(st[0])
        for k in wkeys:
            st = self._state(k)
            if st[0] is not None:
                deps.add(st[0])
            deps.update(st[1].values())
            deps.update(st[2])
        for k in rkeys:
            st = self._state(k)
            if op.is_dma:
                st[2].append(oid)
            else:
                st[1][eng] = oid
        for k in wkeys:
            st = self._state(k)
            st[0] = oid
            st[1] = {}
            st[2] = []
        deps.discard(oid)
        best = {}
        final = []
        for d in deps:
            p = self.ops[d]
            if p.is_dma:
                final.append(d)
            else:
                if p.eng == eng and not op.is_dma:
                    if eng == "pe" or not SAME_ENGINE_SYNC:
                        continue
                if p.eng not in best or best[p.eng] < d:
                    best[p.eng] = d
        final.extend(best.values())
        for d in final:
            self.ops[d].signal = True
        op.deps = final
        if op.is_dma:
            g = self.dma_groups.setdefault(dma, {"sem": None, "n": 0})
            g["n"] += 1
            op.val = 16 * g["n"]
            op.signal = True
        self.ops.append(op)
        self.by_eng[eng].append(oid)
        return oid

    def emit(self, stack):
        nc = self.nc
        esem = {e: stack.enter_context(nc.semaphore("sem_" + e)) for e in ("pe", "act", "dve", "pool")}
        for name, g in self.dma_groups.items():
            g["sem"] = stack.enter_context(nc.semaphore("dma_" + name))
        for e, lst in self.by_eng.items():
            c = 0
            for oid in lst:
                op = self.ops[oid]
                if op.is_dma:
                    continue
                if op.signal:
                    c += 1
                    op.cnt = c
        ops = self.ops
        groups = self.dma_groups

        def token(p):
            if p.is_dma:
                g = groups[p.grp]
                v = 16 * g["n"] if p.grp.startswith("T:") else p.val
                return g["sem"], v
            return esem[p.eng], p.cnt

        def run(engname, eng):
            waited = {}
            lst = self.by_eng[engname]
            for oid in lst:
                op = ops[oid]
                for d in op.deps:
                    sem, v = token(ops[d])
                    key = id(sem)
                    if waited.get(key, 0) >= v:
                        continue
                    waited[key] = v
                    eng.wait_ge(sem, v)
                ins = op.fn(eng)
                if op.is_dma:
                    ins.then_inc(groups[op.grp]["sem"], 16)
                elif op.signal:
                    ins.then_inc(esem[engname], 1)
            for name, g in groups.items():
                pass
            return waited

        block = stack.enter_context(nc.Block())

        @block.tensor
        def _(e):
            run("pe", e)

        @block.scalar
        def _(e):
            run("act", e)

        @block.vector
        def _(e):
            run("dve", e)

        @block.gpsimd
        def _(e):
            run("pool", e)

        @block.sync
        def _(e):
            run("sp", e)
            for name, g in groups.items():
                e.wait_ge(g["sem"], 16 * g["n"])
            for en in ("pe", "act", "dve", "pool"):
                c = 0
                for oid in self.by_eng[en]:
                    if ops[oid].cnt:
                        c = ops[oid].cnt
                if c:
                    e.wait_ge(esem[en], c)


def build_program(debug=False, stop=None):
    from contextlib import ExitStack
    nc = bass.Bass("TRN2", target_bir_lowering=False)
    P = Prog(nc)

    def finish():
        from contextlib import ExitStack as _ES
        with _ES() as stack:
            P.emit(stack)
        return nc

    def din(name, shape, dt=F32):
        return nc.dram_tensor(name, list(shape), dt, kind="ExternalInput").ap()

    x_d = din("x", [S, D])
    ctx_d = din("ctx", [CTX, D])
    cvec_d = din("cvec", [128, 8, 2])
    wmod_d = din("w_mod", [D, 6 * D])
    bmod_d = din("bmod", [128, 48])
    n1g_d = din("n1g", [128, 8])
    n2g_d = din("n2g", [128, 8])
    win_d = din("w_in", [D, 2048])
    sgug_d = din("sgug_rep", [128, 512])
    wsT_d = din("wsT", [128, 4, 128])
    sgub_d = din("sgub", [1, 512])
    convw_d = din("convw", [128, 4, 4])
    convb_d = din("convb", [128, 4])
    wg_d = din("wg", [128, 16, 128])
    gb_d = din("gb", [128, 16])
    lam_d = din("lam", [128, 8])
    wout_d = din("w_out", [D, D])
    wr_d = din("wr", [128, 8, 16])
    w1_d = din("w1", [NE, D, 2048])
    w3_d = din("w3", [NE, D, 2048])
    w2_d = din("w2", [NE, 2048, D])
    fg_d = din("fg_rep", [128, D])
    ident_d = din("ident", [128, 128])
    iota_d = din("iota256", [128, 256])
    ltri_d = din("ltri", [128, 128])
    blk_d = din("blk", [128, 128])
    self_d = din("selfull", [128, 256])
    tinfo_d = din("tinfo", [128, 16, 2])
    out_d = nc.dram_tensor("out", [S, D], F32, kind="ExternalOutput").ap()
    xn2_dram = nc.dram_tensor("xn2_scratch", [S, XROW], BF16, kind="Internal").ap()
    acc_dram = nc.dram_tensor("acc_scratch", [S, D], F32, kind="Internal").ap()
    dbg = {}
    if debug:
        dbg["mixT"] = nc.dram_tensor("dbg_mixT", [128, 8, S], BF16, kind="ExternalOutput").ap()
        dbg["aff"] = nc.dram_tensor("dbg_aff", [128, 16, 16], F32, kind="ExternalOutput").ap()
        dbg["rankm"] = nc.dram_tensor("dbg_rankm", [128, 256], F32, kind="ExternalOutput").ap()
        dbg["mod"] = nc.dram_tensor("dbg_mod", [128, 48, 2], F32, kind="ExternalOutput").ap()
        dbg["x1"] = nc.dram_tensor("dbg_x1", [S, D], F32, kind="ExternalOutput").ap()

    def sb(name, shape, dt, off):
        t = nc.alloc_sbuf_tensor_at(name, list(shape), dt, offset=off)
        return Buf(t, "sb", off, shape, dt)

    class Bump:
        def __init__(self, lo, hi):
            self.p, self.hi = lo, hi

        def __call__(self, name, shape, dt, align=64):
            n = 1
            for s in shape[1:]:
                n *= s
            nb = n * _ESZ[dt]
            self.p = (self.p + align - 1) // align * align
            off = self.p
            self.p += nb
            assert self.p <= self.hi, (name, self.p, self.hi)
            return sb(name, shape, dt, off)

    base = (int(nc.sbuf_base) + 63) // 64 * 64
    arena = nc.alloc_sbuf_tensor("arena", [128, 212800], mybir.dt.uint8)
    OFF_POOL = base
    OFF_MIX = OFF_POOL + NSLOT_LO * SLOT_BYTES
    OFF_HX = OFF_MIX + 32768
    OFF_RG = OFF_HX + 32768
    OFF_MISC = OFF_RG + 51200
    END = base + 212800

    slotA, slotB = [], []
    for s in range(NSLOT):
        if s < NSLOT_LO:
            off = OFF_POOL + s * SLOT_BYTES
        elif s < 12:
            off = OFF_HX + (s - NSLOT_LO) * SLOT_BYTES
        else:
            off = OFF_MISC + (s - 12) * SLOT_BYTES
        slotA.append(sb(f"slotA{s}", [128, 8, 512], BF16, off))
        slotB.append(sb(f"slotB{s}", [128, 4, 1024], BF16, off))

    mixT = sb("mixT", [128, 8, S], BF16, OFF_MIX)
    hxT = sb("hxT", [128, 8, S], BF16, OFF_HX)

    rgb = Bump(OFF_RG, OFF_MISC)
    xxp = rgb("xxp", [128, 32, 67], F32)
    xcp = rgb("xcp", [128, 259], F32)
    cx = rgb("cx", [128, 2304], F32)
    cxb = rgb("cxb", [128, 2304], BF16)
    Rb = rgb("Rb", [128, 2304], F32)
    Sb = rgb("Sb", [128, 2304], F32)
    Ib = rgb("Ib", [128, 2304], F32)
    Hf = sb("Hf", [128, 2304], F32, xxp.off)
    vb = Bump(Rb.off, OFF_MISC)
    gvs = [vb(f"gv{i}", [128, 512], F32) for i in range(4)]
    gsqfs = [vb(f"gsqf{i}", [128, 512], F32) for i in range(2)]
    ss4a = vb("ss4a", [128, 16, 4], F32)
    r4a = vb("r4a", [128, 16, 4], F32)
    vn = [vb(f"vn{i}", [128, 512], BF16) for i in range(2)]
    wb = Bump(Sb.off, OFF_MISC)
    g1x_rep = wb("g1x_rep", [128, D], F32)
    dg = wb("dg", [128, 128], F32)

    mb = Bump(OFF_MISC, END)
    hcT = mb("hcT", [128, 8, CTX], BF16)
    xt = [mb(f"xt{i}", [128, D], F32) for i in range(2)]
    xnb = [mb(f"xnb{i}", [128, D], BF16) for i in range(2)]
    identb = mb("identb", [128, 128], BF16)
    identf = mb("identf", [128, 128], F32)
    onesb = mb("onesb", [128, 128], BF16)
    onesf = mb("onesf", [128, 128], F32)
    wgb = mb("wgb", [128, 16, 128], BF16)
    wsTb = mb("wsTb", [128, 4, 128], BF16)
    sgug = mb("sgug", [128, 512], F32)
    sgubb = mb("sgubb", [1, 512], BF16)
    modsb = mb("modsb", [128, 48, 2], F32)
    cvec = mb("cvec", [128, 8, 2], F32)
    scb = mb("scb", [128, 8, 2], BF16)
    bmod = mb("bmod", [128, 48], F32)
    n1g = mb("n1g", [128, 8], F32)
    n2g = mb("n2g", [128, 8], F32)
    gsc1x = mb("gsc1x", [128, 8], F32)
    gsc1c = mb("gsc1c", [128, 8], F32)
    gsc2x = mb("gsc2x", [128, 8], F32)
    convw = mb("convw", [128, 4, 4], F32)
    convb = mb("convb", [128, 4], F32)
    gbv = mb("gbv", [128, 16], F32)
    lam = mb("lam", [128, 8], F32)
    cl = mb("cl", [128, 8], F32)
    cl2 = mb("cl2", [128, 8], F32)
    ss = mb("ss", [128, 18], F32)
    rs = mb("rs", [128, 18], F32)
    rstd = mb("rstd", [128, 18], F32)
    ss4 = mb("ss4", [128, 4], F32)
    r4 = mb("r4", [128, 4], F32)
    wrf = mb("wrf", [128, 8, 16], F32)
    wrp = mb("wrp", [128, 8, 16], BF16)
    wrb = mb("wrb", [128, 8, 16], BF16)
    sh2b = mb("sh2b", [128, 8], BF16)
    rbiasb = mb("rbiasb", [1, 16], BF16)
    ss2 = mb("ss2", [128, 16], F32)
    rs2 = mb("rs2", [128, 16], F32)
    rstd2 = mb("rstd2", [128, 16], F32)
    smx = mb("smx", [128, 4], F32)
    idx_all = mb("idx_all", [128, NE, 2], I32)
    lo_t = mb("lo_t", [128, 1], F32)
    neghalf = mb("neghalf", [128, 16], F32)
    idxf = mb("idxf", [128, 2], F32)
    mid_t = mb("mid_t", [128, 1], F32)
    cnt2 = mb("cnt2", [128, 2], F32)
    stp = mb("stp", [128, 1], F32)
    ss3 = mb("ss3", [128, 16], F32)
    rs3 = mb("rs3", [128, 16], F32)
    rstd3 = mb("rstd3", [128, 16], F32)

    xb = Bump(OFF_MIX, OFF_HX)
    Se = xb("Se", [128, 16, 256], BF16)
    hidT = xb("hidT", [128, 16, 256], BF16)
    ye = xb("ye", [128, 2, D], F32)
    xeT = xb("xeT", [128, 8, 256], BF16)
    sl = [xb(f"sl{i}", [128, 256], F32) for i in range(2)]
    eb = Bump(OFF_RG, OFF_MISC)
    xg = eb("xg", [128, 2, XROW], BF16)
    g2x_rep = eb("g2x_rep", [128, D], F32)
    fg_rep = eb("fg_rep", [128, D], F32)
    iota256 = eb("iota256", [128, 256], F32)
    rankm = eb("rankm", [128, 256], F32)
    aff_all = eb("aff_all", [128, 16, 16], F32)
    maskf = eb("maskf", [128, 256], F32)
    maskb = eb("maskb", [128, 256], BF16)
    offt = eb("offt", [128, 16, 16], F32)
    rk = eb("rk", [128, 256], F32)
    affg = eb("affg", [128, 2, 128], F32)
    affT = eb("affT", [128, 256], F32)
    cmpj = eb("cmpj", [128, 256], BF16)
    selt = eb("selt", [128, 256], F32)
    ltrib = eb("ltrib", [128, 128], BF16)
    blkf = eb("blkf", [128, 128], F32)
    selfull = eb("selfull", [128, 256], F32)
    tinfob = eb("tinfob", [128, 16, 2], BF16)
    exl = eb("exl", [128, 16], F32)
    x1t = [eb(f"x1t{i}", [128, D], F32) for i in range(2)]
    xn2b = [eb(f"xn2b{i}", [128, D], BF16) for i in range(2)]
    xn2Ts = [eb(f"xn2T{i}", [128, 8, 128], BF16) for i in range(2)]
    exls = [eb(f"exl{i}", [128, 16], F32) for i in range(2)]
    smxa = eb("smxa", [128, 16, 4], F32)
    tail_all = eb("tail_all", [128, 16, 48], BF16)
    dg2 = eb("dg2", [128, 128], F32)
    ob = Bump(OFF_MIX, OFF_HX)
    outt = [ob(f"outt{i}", [128, D], F32) for i in range(4)]
    x2e = [sb(f"x2e{i}", [128, D], F32, ye.off + i * 4096) for i in range(2)]

    assert wgb.off + 8192 == sgubb.off + 1024 and wgb.off % 64 == 0
    assert x1t[0].off + 4096 == x1t[1].off
    assert xn2b[0].off + 2048 == xn2b[1].off and xn2b[1].off + 2048 == xn2Ts[0].off \
        and xn2Ts[0].off + 2048 == xn2Ts[1].off
    for nm, off in (("14", wgb.off), ("15", x1t[0].off), ("16", xn2b[0].off)):
        slotA.append(sb(f"slotA{nm}", [128, 8, 512], BF16, off))
        slotB.append(sb(f"slotB{nm}", [128, 4, 1024], BF16, off))

    def ps(name, shape, dt):
        t = nc.alloc_psum_tensor(name, list(shape), dt)
        return t

    pbank = []
    for i in range(6):
        t = ps(f"pb{i}", [128, 512], F32)
        pbank.append(Buf(t, "ps", i * 2048, [128, 512], F32))
    for b_ in pbank:
        b_.bf = b_.t[:].bitcast(BF16)
    tbanks = pbank[0:4]
    trps = Buf(ps("trps", [128, 8, 128], BF16), "ps", 6 * 2048, [128, 8, 128], BF16)
    pmisc = Buf(ps("pmisc", [128, 512], F32), "ps", 7 * 2048, [128, 512], F32)

    def dma(eng, grp, out_ap, in_ap, reads, writes):
        return P.add(eng, lambda e, o=out_ap, i=in_ap: e.dma_start(out=o, in_=i), reads, writes, dma=grp)

    CONST = "T:const"
    CONSTP = "T:constp"

    def load_const(buf, src):
        dma("sp", CONST, buf[:], src, [], [buf.r()])

    def load_const_cast(buf, src):
        dma("pool", CONSTP, buf[:], src, [], [buf.r()])

    units = []

    def rA(ap):
        return ap.rearrange("(k p) n -> p k n", p=128)

    for j in range(4):
        units.append((rA(wmod_d[:, j * 512:(j + 1) * 512]), "A"))
    for c in (0, 1, 3, 2):
        units.append((rA(win_d[:, c * 512:(c + 1) * 512]), "A"))
    for j in range(4, 12):
        units.append((rA(wmod_d[:, j * 512:(j + 1) * 512]), "A"))
    for n in range(2):
        units.append((rA(wout_d[:, n * 512:(n + 1) * 512]), "A"))
    for e in range(NE):
        for cb in range(4):
            units.append((rA(w1_d[e][:, cb * 512:(cb + 1) * 512]), "A"))
            units.append((rA(w3_d[e][:, cb * 512:(cb + 1) * 512]), "A"))
        for kg in range(4):
            units.append((rA(w2_d[e][kg * 512:(kg + 1) * 512, :]), "B"))

    st = {"next": 0, "free": list(range(NSLOT_LO)), "acq": 0, "slot_of": {}, "hi_open": False}

    def pump():
        while st["next"] < len(units) and st["free"]:
            s = st["free"].pop(0)
            u = st["next"]
            st["next"] += 1
            src, kind = units[u]
            buf = slotA[s] if kind == "A" else slotB[s]
            dma("pool", f"slot{s}", buf[:], src, [], [buf.r()])
            st["slot_of"][u] = s

    def acquire():
        u = st["acq"]
        st["acq"] += 1
        if u not in st["slot_of"]:
            pump()
        s = st["slot_of"][u]
        kind = units[u][1]
        return s, (slotA[s] if kind == "A" else slotB[s])

    def release(s):
        st["free"].append(s)
        pump()

    def open_hi_slots():
        if not st["hi_open"]:
            st["hi_open"] = True
            st["free"].extend(list(range(NSLOT_LO, 12)) + [15, 16])
            pump()

    load_const(cvec, cvec_d)
    load_const(bmod, bmod_d)
    load_const(n1g, n1g_d)
    load_const(n2g, n2g_d)
    load_const(identf, ident_d)
    load_const(sgug, sgug_d)
    load_const(convw, convw_d)
    load_const(convb, convb_d)
    load_const(gbv, gb_d)
    load_const(lam, lam_d)
    load_const(wrf, wr_d)
    load_const_cast(identb, ident_d)
    load_const_cast(wsTb, wsT_d)
    load_const_cast(wgb, wg_d)
    load_const_cast(sgubb, sgub_d)
    pump()

    if stop == "c0":
        return finish()
    P.add("pool", lambda e: e.memset(neghalf[:], -0.5), [], [neghalf.r()])

    def rstd_op(dst, src, lo, hi, inv_n):
        w = hi - lo
        P.add("pool", lambda e: e.tensor_scalar(out=dst[:].rearrange("p ... -> p (...)")[:, lo:hi] if False else _flat(dst)[:, lo:hi],
                                                in0=_flat(src)[:, lo:hi], scalar1=inv_n, scalar2=EPS,
                                                op0=ALU.mult, op1=ALU.add), [src.r(lo, hi)], [dst.r(lo, hi)])
        P.add("pool", lambda e: e.tensor_tensor(out=_flat(dst)[:, lo:hi], in0=_flat(dst)[:, lo:hi],
                                                in1=neghalf[:, 0:w], op=ALU.pow),
              [dst.r(lo, hi), neghalf.r()], [dst.r(lo, hi)])

    def _flat(buf):
        if len(buf.shape) == 2:
            return buf[:]
        return buf[:].rearrange("p a b -> p (a b)")

    P.add("pool", lambda e: e.memset(onesb[:], 1.0), [], [onesb.r()])
    P.add("pool", lambda e: e.memset(onesf[:], 1.0), [], [onesf.r()])
    P.add("act", lambda e: e.activation(out=scb[:], in_=cvec[:], func=AF.Silu), [cvec.r()], [scb.r()])
    P.add("act", lambda e: e.activation(out=cl[:], in_=lam[:], func=AF.Exp, scale=-1.0), [lam.r()], [cl.r()])
    P.add("act", lambda e: e.activation(out=cl[:], in_=cl[:], func=AF.Ln, bias=1.0, scale=1.0), [cl.r()], [cl.r()])
    P.add("dve", lambda e: e.tensor_scalar(out=cl2[:], in0=cl[:], scalar1=-16.0, scalar2=None, op0=ALU.mult),
          [cl.r()], [cl2.r()])
    P.add("dve", lambda e: e.tensor_scalar(out=cl[:], in0=cl[:], scalar1=-8.0, scalar2=None, op0=ALU.mult),
          [cl.r()], [cl.r()])

    if stop == "c1":
        return finish()
    modps = pmisc

    def mod_unit(j):
        s, buf = acquire()
        for mm in range(4):
            m = j * 4 + mm
            for k in range(8):
                P.add("pe", lambda e, buf=buf, mm=mm, m=m, k=k: e.matmul(
                    pmisc[:, 2 * m:2 * m + 2], lhsT=buf[:, k, mm * 128:(mm + 1) * 128], rhs=scb[:, k, :],
                    start=(k == 0), stop=(k == 7)),
                    [buf.r3(k, mm * 128, (mm + 1) * 128), scb.r()], [pmisc.r(2 * m, 2 * m + 2)])
        m0 = j * 4
        if stop == "m0":
            return
        P.add("dve", lambda e, m0=m0: e.tensor_tensor(
            out=modsb[:, m0:m0 + 4, :], in0=pmisc[:, 2 * m0:2 * m0 + 8].rearrange("p (m t) -> p m t", t=2),
            in1=bmod[:, m0:m0 + 4].unsqueeze(2).broadcast_to([128, 4, 2]), op=ALU.add),
            [pmisc.r(2 * m0, 2 * m0 + 8), bmod.r()], [modsb.r(2 * m0, 2 * m0 + 8)])
        release(s)

    for j in range(4):
        mod_unit(j)
    if stop in ("m0", "m1"):
        return finish()

    def mod_col(m0, which):
        return modsb[:, m0:m0 + 8, which]

    def mod1(m, which):
        return modsb[:, m, which:which + 1]

    P.add("dve", lambda e: e.scalar_tensor_tensor(out=gsc1x[:], in0=mod_col(8, 0), scalar=1.0, in1=n1g[:],
                                                  op0=ALU.add, op1=ALU.mult), [modsb.r(), n1g.r()], [gsc1x.r()])
    P.add("dve", lambda e: e.scalar_tensor_tensor(out=gsc1c[:], in0=mod_col(8, 1), scalar=1.0, in1=n1g[:],
                                                  op0=ALU.add, op1=ALU.mult), [modsb.r(), n1g.r()], [gsc1c.r()])

    if stop == "prologue":
        return finish()
    def phase_a_tile(src_rows, col, dstT, c0, gsc, which, par):
        xtb, xn = xt[par], xnb[par]
        dma("sp", f"xt{par}", xtb[:], src_rows, [], [xtb.r()])
        P.add("act", lambda e: e.activation(out=xn[:], in_=xtb[:], func=AF.Square, accum_out=ss[:, col:col + 1]),
              [xtb.r()], [xn.r(), ss.r(col, col + 1)])
        if stop == "a0":
            return
        rstd_op(rstd, ss, col, col + 1, 1.0 / D)
        if stop == "a1":
            return
        P.add("act", lambda e: e.activation(out=xn[:], in_=xtb[:], func=AF.Copy, scale=rstd[:, col:col + 1]),
              [xtb.r(), rstd.r(col, col + 1)], [xn.r()])
        if stop == "a2":
            return
        tb_ = tbanks[col % len(tbanks)]
        tv = tb_.bf
        for k in range(8):
            P.add("pe", lambda e, k=k: e.transpose(out=tv[:, k * 128:(k + 1) * 128],
                                                   in_=xn[:, k * 128:(k + 1) * 128], identity=identb[:]),
                  [xn.r(k * 128, (k + 1) * 128), identb.r()], [tb_.r()])
        if stop == "a3":
            return
        for k in range(8):
            if col % 2 == 0 or EVAC_ACT_ONLY:
                P.add("act", lambda e, k=k: e.activation(out=dstT[:, k, c0:c0 + 128],
                                                         in_=tv[:, k * 128:(k + 1) * 128],
                                                         func=AF.Identity, scale=gsc[:, k:k + 1],
                                                         bias=mod1(k, which)),
                      [tb_.r(), gsc.r(), modsb.r()], [dstT.r3(k, c0, c0 + 128)])
            else:
                P.add("dve", lambda e, k=k: e.tensor_scalar(out=dstT[:, k, c0:c0 + 128],
                                                            in0=tv[:, k * 128:(k + 1) * 128],
                                                            scalar1=gsc[:, k:k + 1], scalar2=mod1(k, which),
                                                            op0=ALU.mult, op1=ALU.add),
                      [tb_.r(), gsc.r(), modsb.r()], [dstT.r3(k, c0, c0 + 128)])

    tcount = 0
    for i in range(2):
        phase_a_tile(ctx_d[i * 128:(i + 1) * 128, :], i, hcT, i * 128, gsc1c, 1, tcount % 2)
        tcount += 1
        if stop in ("a0", "a1", "a2", "a3", "a4"):
            return finish()
    for i in range(16):
        phase_a_tile(x_d[i * 128:(i + 1) * 128, :], 2 + i, hxT, i * 128, gsc1x, 0, tcount % 2)
        tcount += 1

    if stop == "A":
        return finish()
    bank_rr = [0]

    def next_bank():
        b = pbank[bank_rr[0] % 4]
        bank_rr[0] += 1
        return b

    def proj_feature_major(buf, m, src, tb0, ncols, pb):
        for k in range(8):
            P.add("pe", lambda e, k=k: e.matmul(pb[:, 0:ncols], lhsT=buf[:, k, m * 128:(m + 1) * 128],
                                                rhs=src[:, k, tb0:tb0 + ncols], start=(k == 0), stop=(k == 7)),
                  [buf.r3(k, m * 128, (m + 1) * 128), src.r3(k, tb0, tb0 + ncols)], [pb.r(0, ncols)])

    s_u, bu = acquire()
    for m in range(4):
        for tb in range(4):
            pb = next_bank()
            proj_feature_major(bu, m, hxT, tb * 512, 512, pb)
            P.add("act", lambda e, m=m, tb=tb, pb=pb: e.activation(out=mixT[:, m, tb * 512:(tb + 1) * 512],
                                                                    in_=pb[:, :], func=AF.Gelu_apprx_tanh),
                  [pb.r()], [mixT.r3(m, tb * 512, (tb + 1) * 512)])
    release(s_u)

    if stop == "u":
        return finish()
    s_v, bv = acquire()
    sps = [pbank[4], pbank[5]]
    def v_stage1(i):
        pb = next_bank()
        for k in range(8):
            P.add("pe", lambda e, k=k, i=i, pb=pb: e.matmul(pb[:, :], lhsT=hxT[:, k, i * 128:(i + 1) * 128],
                                                            rhs=bv[:, k, :], start=(k == 0), stop=(k == 7)),
                  [hxT.r3(k, i * 128, (i + 1) * 128), bv.r3(k, 0, 512)], [pb.r()])
        gv = gvs[i % 4]
        P.add("act", lambda e, pb=pb, gv=gv: e.activation(out=gv[:], in_=pb[:, :], func=AF.Gelu_apprx_tanh),
              [pb.r()], [gv.r()])
        P.add("act", lambda e, gv=gv, i=i: e.activation(out=gsqfs[i % 2][:], in_=gv[:], func=AF.Square), [gv.r()],
              [gsqfs[i % 2].r()])

    def v_stage1b(i):
        P.add("dve", lambda e, i=i: e.tensor_reduce(out=ss4a[:, i, :], in_=gsqfs[i % 2][:].rearrange("p (h c) -> p h c", h=4),
                                                    axis=AX.X, op=ALU.add), [gsqfs[i % 2].r()], [ss4a.r3(i, 0, 4)])
        rstd_op(r4a, ss4a, i * 4, i * 4 + 4, 1.0 / 128)

    def v_stage2(i):
        gv = gvs[i % 4]
        vnb = vn[i % 2]
        for h in range(4):
            P.add("dve", lambda e, h=h, i=i, vnb=vnb, gv=gv: e.scalar_tensor_tensor(
                out=vnb[:, h * 128:(h + 1) * 128], in0=gv[:, h * 128:(h + 1) * 128], scalar=r4a[:, i, h:h + 1],
                in1=sgug[:, h * 128:(h + 1) * 128], op0=ALU.mult, op1=ALU.mult),
                [gv.r(h * 128, (h + 1) * 128), r4a.r3(i, 0, 4), sgug.r()], [vnb.r(h * 128, (h + 1) * 128)])

    def v_stage3(i):
        vnb = vn[i % 2]
        sp_ = sps[i % 2]
        for h in range(4):
            P.add("pe", lambda e, h=h, vnb=vnb, sp_=sp_: e.matmul(sp_[:, h * 128:(h + 1) * 128],
                                                                  lhsT=vnb[:, h * 128:(h + 1) * 128],
                                                                  rhs=wsTb[:, h, :], start=True, stop=False),
                  [vnb.r(h * 128, (h + 1) * 128), wsTb.r()], [sp_.r(h * 128, (h + 1) * 128)])
            P.add("pe", lambda e, h=h, sp_=sp_: e.matmul(sp_[:, h * 128:(h + 1) * 128], lhsT=onesb[0:1, :],
                                                         rhs=sgubb[0:1, h * 128:(h + 1) * 128],
                                                         start=False, stop=True),
                  [onesb.r(), sgubb.r()], [sp_.r(h * 128, (h + 1) * 128)])
        P.add("dve", lambda e, i=i, sp_=sp_: e.tensor_tensor(
            out=mixT[:, 0:4, i * 128:(i + 1) * 128], in0=mixT[:, 0:4, i * 128:(i + 1) * 128],
            in1=sp_[:, :].rearrange("p (h c) -> p h c", h=4), op=ALU.mult),
            [sp_.r()] + mixT.rks(range(4), i * 128, (i + 1) * 128), mixT.rks(range(4), i * 128, (i + 1) * 128))

    for t in range(16 + 3):
        if t < 16:
            v_stage1(t)
        if 0 <= t - 1 < 16:
            v_stage1b(t - 1)
        if 0 <= t - 2 < 16:
            v_stage2(t - 2)
        if 0 <= t - 3 < 16:
            v_stage3(t - 3)
    release(s_v)

    if stop == "v":
        return finish()
    s_g, bg = acquire()
    for m in range(4):
        for tb in range(4):
            pb = next_bank()
            proj_feature_major(bg, m, hxT, tb * 512, 512, pb)
            P.add("act", lambda e, m=m, tb=tb, pb=pb: e.activation(out=mixT[:, 4 + m, tb * 512:(tb + 1) * 512],
                                                                    in_=pb[:, :], func=AF.Gelu_apprx_tanh),
                  [pb.r()], [mixT.r3(4 + m, tb * 512, (tb + 1) * 512)])
    release(s_g)

    if stop == "g":
        return finish()
    s_x, bx = acquire()
    for cc in range(4):
        P.add("pool", lambda e: e.memset(xxp[:, :, 0:1], 0.0), [], [xxp.r()])
        P.add("pool", lambda e: e.memset(xxp[:, :, 65:67], 0.0), [], [xxp.r()])
        P.add("pool", lambda e: e.memset(xcp[:, 0:1], 0.0), [], [xcp.r(0, 1)])
        P.add("pool", lambda e: e.memset(xcp[:, 257:259], 0.0), [], [xcp.r(257, 259)])
        pb = next_bank()
        proj_feature_major(bx, cc, hcT, 0, CTX, pb)
        P.add("act", lambda e, pb=pb: e.activation(out=xcp[:, 1:257], in_=pb[:, 0:CTX], func=AF.Copy),
              [pb.r(0, CTX)], [xcp.r(1, 257)])
        for tb in range(4):
            pb = next_bank()
            proj_feature_major(bx, cc, hxT, tb * 512, 512, pb)
            P.add("act", lambda e, tb=tb, pb=pb: e.activation(
                out=xxp[:, tb * 8:(tb + 1) * 8, 1:65], in_=pb[:, :].rearrange("p (r c) -> p r c", c=64),
                func=AF.Copy), [pb.r()], [xxp.r(tb * 8 * 67, (tb + 1) * 8 * 67)])
        cx_lat = cx[:, 256:2304].rearrange("p (r c) -> p r c", c=64)
        P.add("act", lambda e, cc=cc: e.activation(out=cx_lat, in_=xxp[:, :, 0:64], func=AF.Identity,
                                                   scale=convw[:, cc, 0:1], bias=convb[:, cc:cc + 1]),
              [xxp.r(), convw.r(), convb.r()], [cx.r(256, 2304)])
        P.add("act", lambda e, cc=cc: e.activation(out=cx[:, 0:256], in_=xcp[:, 0:256], func=AF.Identity,
                                                   scale=convw[:, cc, 0:1], bias=convb[:, cc:cc + 1]),
              [xcp.r(), convw.r(), convb.r()], [cx.r(0, 256)])
        for k in range(1, 4):
            P.add("dve", lambda e, cc=cc, k=k: e.scalar_tensor_tensor(
                out=cx_lat, in0=xxp[:, :, k:k + 64], scalar=convw[:, cc, k:k + 1], in1=cx_lat,
                op0=ALU.mult, op1=ALU.add), [xxp.r(), convw.r(), cx.r(256, 2304)], [cx.r(256, 2304)])
            P.add("dve", lambda e, cc=cc, k=k: e.scalar_tensor_tensor(
                out=cx[:, 0:256], in0=xcp[:, k:k + 256], scalar=convw[:, cc, k:k + 1], in1=cx[:, 0:256],
                op0=ALU.mult, op1=ALU.add), [xcp.r(), convw.r(), cx.r(0, 256)], [cx.r(0, 256)])
        P.add("dve", lambda e: e.tensor_copy(out=cxb[:], in_=cx[:]), [cx.r()], [cxb.r()])
        for d in range(2):
            if d == 0:
                blocks = [(0, 256, 0)] + [(256 + t * 512, 512, 256 + t * 512) for t in range(4)]
            else:
                blocks = [(0, 256, 2048)] + [(256 + t * 512, 512, t * 512) for t in range(4)]
            ga = (d * 2 + 0) * 4 + cc
            gi = (d * 2 + 1) * 4 + cc
            for (s0, n, d0) in blocks:
                pb = next_bank()
                P.add("pe", lambda e, ga=ga, s0=s0, n=n, pb=pb: e.matmul(pb[:, 0:n], lhsT=wgb[:, ga, :],
                                                                         rhs=cxb[:, s0:s0 + n],
                                                                         start=True, stop=True),
                      [wgb.r(), cxb.r(s0, s0 + n)], [pb.r(0, n)])
                P.add("act", lambda e, ga=ga, n=n, d0=d0, pb=pb: e.activation(
                    out=Rb[:, d0:d0 + n], in_=pb[:, 0:n], func=AF.Sigmoid, bias=gbv[:, ga:ga + 1], scale=1.0),
                    [pb.r(0, n), gbv.r()], [Rb.r(d0, d0 + n)])
            for (s0, n, d0) in blocks:
                pb = next_bank()
                P.add("pe", lambda e, gi=gi, s0=s0, n=n, pb=pb: e.matmul(pb[:, 0:n], lhsT=wgb[:, gi, :],
                                                                         rhs=cxb[:, s0:s0 + n],
                                                                         start=True, stop=True),
                      [wgb.r(), cxb.r(s0, s0 + n)], [pb.r(0, n)])
                P.add("act", lambda e, gi=gi, n=n, d0=d0, pb=pb: e.activation(
                    out=Ib[:, d0:d0 + n], in_=pb[:, 0:n], func=AF.Sigmoid, bias=gbv[:, gi:gi + 1], scale=1.0),
                    [pb.r(0, n), gbv.r()], [Ib.r(d0, d0 + n)])
            ci = d * 4 + cc
            P.add("act", lambda e, ci=ci: e.activation(out=Sb[:], in_=Rb[:], func=AF.Exp,
                                                       scale=cl2[:, ci:ci + 1]), [Rb.r(), cl2.r()], [Sb.r()])
            P.add("act", lambda e, ci=ci: e.activation(out=Rb[:], in_=Rb[:], func=AF.Exp,
                                                       scale=cl[:, ci:ci + 1]), [Rb.r(), cl.r()], [Rb.r()])
            P.add("act", lambda e: e.activation(out=Sb[:], in_=Sb[:], func=AF.Sqrt, bias=1.0, scale=-1.0),
                  [Sb.r()], [Sb.r()])
            P.add("dve", lambda e: e.tensor_tensor(out=Sb[:], in0=Sb[:], in1=Ib[:], op=ALU.mult),
                  [Sb.r(), Ib.r()], [Sb.r()])
            if d == 0:
                P.add("dve", lambda e: e.tensor_tensor(out=Sb[:], in0=Sb[:], in1=cx[:], op=ALU.mult),
                      [Sb.r(), cx.r()], [Sb.r()])
                P.add("dve", lambda e: e.tensor_tensor_scan(out=Hf[:], data0=Rb[:], data1=Sb[:], initial=0.0,
                                                            op0=ALU.mult, op1=ALU.add),
                      [Rb.r(), Sb.r()], [Hf.r()])
            else:
                P.add("dve", lambda e: e.tensor_tensor(out=Sb[:, 0:2048], in0=Sb[:, 0:2048], in1=cx[:, 256:2304],
                                                       op=ALU.mult), [Sb.r(0, 2048), cx.r(256, 2304)],
                      [Sb.r(0, 2048)])
                P.add("dve", lambda e: e.tensor_tensor(out=Sb[:, 2048:2304], in0=Sb[:, 2048:2304],
                                                       in1=cx[:, 0:256], op=ALU.mult),
                      [Sb.r(2048, 2304), cx.r(0, 256)], [Sb.r(2048, 2304)])
                P.add("dve", lambda e: e.tensor_tensor_scan(out=Ib[:, ::-1], data0=Rb[:, ::-1], data1=Sb[:, ::-1],
                                                            initial=0.0, op0=ALU.mult, op1=ALU.add),
                      [Rb.r(), Sb.r()], [Ib.r()])
        P.add("dve", lambda e: e.tensor_tensor(out=Hf[:, 256:2304], in0=Hf[:, 256:2304], in1=Ib[:, 0:2048],
                                               op=ALU.add), [Hf.r(256, 2304), Ib.r(0, 2048)], [Hf.r(256, 2304)])
        P.add("dve", lambda e, cc=cc: e.tensor_tensor(out=mixT[:, 4 + cc, :], in0=mixT[:, 4 + cc, :],
                                                      in1=Hf[:, 256:2304], op=ALU.mult),
              [mixT.r3(4 + cc, 0, S), Hf.r(256, 2304)], [mixT.r3(4 + cc, 0, S)])
    release(s_x)
    st["free"].extend([12, 13, 14])
    pump()
    if debug:
        dma("sp", "dbg_mixT", dbg["mixT"], mixT[:], [mixT.r()], [])

    if stop == "RG":
        return finish()
    for j in range(4, 12):
        mod_unit(j)
    if debug:
        dma("sp", "dbg_mod", dbg["mod"], modsb[:], [modsb.r()], [])
    P.add("dve", lambda e: e.scalar_tensor_tensor(out=gsc2x[:], in0=mod_col(32, 0), scalar=1.0, in1=n2g[:],
                                                  op0=ALU.add, op1=ALU.mult), [modsb.r(), n2g.r()], [gsc2x.r()])

    def build_rep(rep, m0, dgb):
        for k in range(8):
            P.add("dve", lambda e, k=k: e.tensor_scalar(out=dgb[:], in0=identf[:], scalar1=mod1(m0 + k, 0),
                                                        scalar2=None, op0=ALU.mult),
                  [identf.r(), modsb.r()], [dgb.r()])
            pb = next_bank()
            P.add("pe", lambda e, pb=pb: e.matmul(pb[:, 0:128], lhsT=onesf[:], rhs=dgb[:], start=True, stop=True),
                  [onesf.r(), dgb.r()], [pb.r(0, 128)])
            P.add("act", lambda e, k=k, pb=pb: e.activation(out=rep[:, k * 128:(k + 1) * 128], in_=pb[:, 0:128],
                                                            func=AF.Copy), [pb.r(0, 128)],
                  [rep.r(k * 128, (k + 1) * 128)])

    build_rep(g1x_rep, 16, dg)
    wo = []
    for n in range(2):
        s, b = acquire()
        wo.append((s, b))
        eng = "dve" if n == 0 else "pool"
        for k in range(8):
            P.add(eng, lambda e, b=b, k=k, n=n: e.tensor_tensor(out=b[:, k, :], in0=b[:, k, :],
                                                                in1=g1x_rep[:, n * 512:(n + 1) * 512],
                                                                op=ALU.mult),
                  [b.r3(k, 0, 512), g1x_rep.r(n * 512, (n + 1) * 512)], [b.r3(k, 0, 512)])

    load_c2 = lambda buf, src: dma("sp", "T:const2", buf[:], src, [], [buf.r()])
    load_c2(fg_rep, fg_d)
    load_c2(iota256, iota_d)
    load_c2(blkf, blk_d)
    load_c2(selfull, self_d)
    dma("pool", "T:constp2", ltrib[:], ltri_d, [], [ltrib.r()])
    dma("pool", "T:constp2", tinfob[:], tinfo_d, [], [tinfob.r()])
    build_rep(g2x_rep, 40, dg2)
    P.add("dve", lambda e: e.tensor_tensor(out=wrp[:], in0=wrf[:],
                                           in1=gsc2x[:].unsqueeze(2).broadcast_to([128, 8, 16]), op=ALU.mult),
          [wrf.r(), gsc2x.r()], [wrp.r()])
    P.add("dve", lambda e: e.tensor_copy(out=wrb[:], in_=wrf[:]), [wrf.r()], [wrb.r()])
    P.add("dve", lambda e: e.tensor_copy(out=sh2b[:], in_=mod_col(24, 0)), [modsb.r()], [sh2b.r()])
    for k in range(8):
        P.add("pe", lambda e, k=k: e.matmul(pmisc[0:1, 0:16], lhsT=sh2b[:, k:k + 1], rhs=wrb[:, k, :],
                                            start=(k == 0), stop=(k == 7)),
              [sh2b.r(), wrb.r()], [pmisc.r(0, 16)])
    P.add("dve", lambda e: e.tensor_copy(out=rbiasb[:], in_=pmisc[0:1, 0:16]), [pmisc.r(0, 16)], [rbiasb.r()])

    if stop == "prep":
        return finish()
    lgb = [pmisc, Buf(trps.t[:].bitcast(F32), "ps", trps.off, [128, 512], F32)]
    lgb[1].t = None
    lgv = [pmisc[:, 0:16], trps.t[:].bitcast(F32).rearrange("p a b -> p (a b)")[:, 0:16]]
    trb = [pbank[4], pbank[5]]
    def cd_vars(i):
        par = i % 2
        return par, x1t[par], xn2b[par], xn2Ts[par], lgv[par], lgb[par], exls[par], trb[par]

    def cd_stage1(i):
        par, x1, xn2, xT, lg, lgB, exl_, tb_ = cd_vars(i)
        if i == 0:
            dma("sp", "x1t0", x1t[0][:], x_d[0:128, :], [], [x1t[0].r()])
        if i + 1 < 16:
            j = i + 1
            dma("sp", f"x1t{j % 2}", x1t[j % 2][:], x_d[j * 128:(j + 1) * 128, :], [], [x1t[j % 2].r()])
        for n in range(2):
            pb = next_bank()
            wb_ = wo[n][1]
            for k in range(8):
                P.add("pe", lambda e, k=k, i=i, pb=pb, wb_=wb_: e.matmul(
                    pb[:, :], lhsT=mixT[:, k, i * 128:(i + 1) * 128], rhs=wb_[:, k, :],
                    start=(k == 0), stop=(k == 7)),
                    [mixT.r3(k, i * 128, (i + 1) * 128), wb_.r3(k, 0, 512)], [pb.r()])
            P.add("dve", lambda e, n=n, pb=pb, x1=x1: e.tensor_tensor(out=x1[:, n * 512:(n + 1) * 512],
                                                                      in0=x1[:, n * 512:(n + 1) * 512],
                                                                      in1=pb[:, :], op=ALU.add),
                  [x1.r(n * 512, (n + 1) * 512), pb.r()], [x1.r(n * 512, (n + 1) * 512)])
        dma("sp", f"accw{par}", acc_dram[i * 128:(i + 1) * 128, :], x1[:], [x1.r()], [("dram", "acc", i)])
        if debug:
            dma("sp", f"dbg_x1{par}", dbg["x1"][i * 128:(i + 1) * 128, :], x1[:], [x1.r()], [])
        P.add("act", lambda e, i=i, x1=x1, xn2=xn2: e.activation(out=xn2[:, 0:D], in_=x1[:], func=AF.Square,
                                                                 accum_out=ss2[:, i:i + 1]),
              [x1.r()], [xn2.r(0, D), ss2.r(i, i + 1)])
        rstd_op(rstd2, ss2, i, i + 1, 1.0 / D)
        P.add("act", lambda e, i=i, x1=x1, xn2=xn2: e.activation(out=xn2[:, 0:D], in_=x1[:], func=AF.Copy,
                                                                 scale=rstd2[:, i:i + 1]),
              [x1.r(), rstd2.r(i, i + 1)], [xn2.r(0, D)])
        dma("sp", f"xn2w{par}", xn2_dram[i * 128:(i + 1) * 128, 0:D], xn2[:, 0:D], [xn2.r(0, D)],
            [("dram", "xn2", i)])

    def cd_stage2(i):
        par, x1, xn2, xT, lg, lgB, exl_, tb_ = cd_vars(i)
        for k in range(8):
            P.add("pe", lambda e, k=k, xn2=xn2, tb_=tb_: e.transpose(
                out=tb_.bf[:, k * 128:(k + 1) * 128], in_=xn2[:, k * 128:(k + 1) * 128], identity=identb[:]),
                [xn2.r(k * 128, (k + 1) * 128), identb.r()], [tb_.r()])
        P.add("act", lambda e, tb_=tb_, xT=xT: e.activation(out=xT[:].rearrange("p a b -> p (a b)"),
                                                            in_=tb_.bf[:, 0:D], func=AF.Copy),
              [tb_.r()], [xT.r()])

    def cd_stage3(i):
        par, x1, xn2, xT, lg, lgB, exl_, tb_ = cd_vars(i)
        for k in range(8):
            P.add("pe", lambda e, k=k, xT=xT, lg=lg: e.matmul(lg, lhsT=xT[:, k, :], rhs=wrp[:, k, :],
                                                              start=(k == 0), stop=False),
                  [xT.r3(k, 0, 128), wrp.r()], [lgB.r(0, 16)])
        P.add("pe", lambda e, lg=lg: e.matmul(lg, lhsT=onesb[0:1, :], rhs=rbiasb[0:1, :],
                                              start=False, stop=True), [onesb.r(), rbiasb.r()], [lgB.r(0, 16)])
        P.add("dve", lambda e, i=i, lg=lg: e.tensor_reduce(out=smxa[:, i, 0:1], in_=lg, axis=AX.X, op=ALU.max),
              [lgB.r(0, 16)], [smxa.r3(i, 0, 1)])
        P.add("dve", lambda e, i=i: e.tensor_scalar(out=smxa[:, i, 1:2], in0=smxa[:, i, 0:1], scalar1=-1.0,
                                                    scalar2=None, op0=ALU.mult), [smxa.r3(i, 0, 1)],
              [smxa.r3(i, 1, 2)])
        P.add("act", lambda e, i=i, lg=lg, exl_=exl_: e.activation(out=exl_[:], in_=lg, func=AF.Exp,
                                                                   bias=smxa[:, i, 1:2], scale=1.0,
                                                                   accum_out=smxa[:, i, 2:3]),
              [lgB.r(0, 16), smxa.r3(i, 1, 2)], [exl_.r(), smxa.r3(i, 2, 3)])
        P.add("dve", lambda e, i=i: e.reciprocal(out=smxa[:, i, 3:4], in_=smxa[:, i, 2:3]), [smxa.r3(i, 2, 3)],
              [smxa.r3(i, 3, 4)])
        P.add("dve", lambda e, i=i, exl_=exl_: e.tensor_scalar(out=aff_all[:, i, :], in0=exl_[:],
                                                               scalar1=smxa[:, i, 3:4], scalar2=None,
                                                               op0=ALU.mult),
              [exl_.r(), smxa.r3(i, 3, 4)], [aff_all.r3(i, 0, 16)])

    for t in range(16 + 2):
        if t < 16:
            cd_stage1(t)
        if 0 <= t - 1 < 16:
            cd_stage2(t - 1)
        if 0 <= t - 2 < 16:
            cd_stage3(t - 2)
    aff_flat0 = aff_all[:].rearrange("p i e -> p (i e)")
    tl3 = tail_all[:].rearrange("p i (e t) -> p (i e) t", t=3)
    P.add("dve", lambda e: e.tensor_copy(out=tl3[:, :, 0], in_=aff_flat0), [aff_all.r()], [tail_all.r()])
    P.add("dve", lambda e: e.tensor_tensor(out=rk[:], in0=aff_flat0, in1=tl3[:, :, 0], op=ALU.subtract),
          [aff_all.r(), tail_all.r()], [rk.r()])
    P.add("dve", lambda e: e.tensor_copy(out=tl3[:, :, 1], in_=rk[:]), [rk.r()], [tail_all.r()])
    P.add("dve", lambda e: e.tensor_tensor(out=rk[:], in0=rk[:], in1=tl3[:, :, 1], op=ALU.subtract),
          [rk.r(), tail_all.r()], [rk.r()])
    P.add("dve", lambda e: e.tensor_copy(out=tl3[:, :, 2], in_=rk[:]), [rk.r()], [tail_all.r()])
    dma("sp", "tailw", xn2_dram.rearrange("(i p) c -> p i c", p=128)[:, :, D:XROW], tail_all[:],
        [tail_all.r()], [("dram", "xn2", i) for i in range(16)])
    release(wo[0][0])
    release(wo[1][0])
    open_hi_slots()
    if debug:
        dma("sp", "dbg_aff", dbg["aff"], aff_all[:], [aff_all.r()], [])

    if stop == "D":
        return finish()
    for h in range(2):
        P.add("dve", lambda e, h=h: e.tensor_copy(
            out=affg[:, h, :].rearrange("p (e s) -> p e s", s=8),
            in_=aff_all[:, h::2, :].rearrange("p s e -> p e s")), [aff_all.r()], [affg.r3(h, 0, 128)])
        P.add("pe", lambda e, h=h: e.transpose(out=pmisc[:, 128 + h * 128:256 + h * 128], in_=affg[:, h, :],
                                               identity=identf[:]),
              [affg.r3(h, 0, 128), identf.r()], [pmisc.r(128 + h * 128, 256 + h * 128)])
    P.add("dve", lambda e: e.tensor_copy(out=affT[:], in_=pmisc[:, 128:384]), [pmisc.r(128, 384)], [affT.r()])
    P.add("dve", lambda e: e.memset(lo_t[:], 0.0), [], [lo_t.r()])
    P.add("dve", lambda e: e.memset(cnt2[:], 0.0), [], [cnt2.r()])
    for it in range(NBISECT):
        hk = 2.0 ** (-(it + 1))
        P.add("dve", lambda e, hk=hk: e.tensor_scalar(out=mid_t[:], in0=lo_t[:], scalar1=hk, scalar2=None,
                                                      op0=ALU.add), [lo_t.r()], [mid_t.r()])
        P.add("dve", lambda e: e.tensor_scalar(out=cmpj[:], in0=affT[:], scalar1=mid_t[:, 0:1], scalar2=None,
                                               op0=ALU.is_ge, op1=ALU.add, accum_out=cnt2[:, 0:1]),
              [affT.r(), mid_t.r()], [cmpj.r(), cnt2.r(0, 1)])
        P.add("pe", lambda e: e.matmul(pmisc[:, 0:2], lhsT=blkf[:], rhs=cnt2[:], start=True, stop=True),
              [blkf.r(), cnt2.r()], [pmisc.r(0, 2)])
        P.add("dve", lambda e, hk=hk: e.tensor_scalar(out=stp[:], in0=pmisc[:, 0:1], scalar1=CAP - 0.5,
                                                      scalar2=hk, op0=ALU.is_ge, op1=ALU.mult),
              [pmisc.r(0, 1)], [stp.r()])
        P.add("dve", lambda e: e.tensor_tensor(out=lo_t[:], in0=lo_t[:], in1=stp[:], op=ALU.add),
              [lo_t.r(), stp.r()], [lo_t.r()])
    P.add("dve", lambda e: e.tensor_scalar(out=selt[:], in0=selfull[:], scalar1=lo_t[:, 0:1], scalar2=None,
                                           op0=ALU.mult), [selfull.r(), lo_t.r()], [selt.r()])
    thr_ps = pbank[0]
    P.add("pe", lambda e: e.matmul(thr_ps[:, 0:256], lhsT=onesf[:], rhs=selt[:], start=True, stop=True),
          [onesf.r(), selt.r()], [thr_ps.r(0, 256)])
    aff_flat = aff_all[:].rearrange("p i e -> p (i e)")
    P.add("dve", lambda e: e.tensor_tensor(out=maskf[:], in0=aff_flat, in1=thr_ps[:, 0:256], op=ALU.is_ge),
          [aff_all.r(), thr_ps.r(0, 256)], [maskf.r()])
    P.add("dve", lambda e: e.tensor_copy(out=maskb[:], in_=maskf[:]), [maskf.r()], [maskb.r()])
    tot_ps = pbank[1]
    rank_ps = pbank[2]
    P.add("pe", lambda e: e.matmul(tot_ps[:, 0:256], lhsT=onesb[:], rhs=maskb[:], start=True, stop=True),
          [onesb.r(), maskb.r()], [tot_ps.r(0, 256)])
    P.add("pe", lambda e: e.matmul(rank_ps[:, 0:256], lhsT=ltrib[:], rhs=maskb[:], start=True, stop=True),
          [ltrib.r(), maskb.r()], [rank_ps.r(0, 256)])
    P.add("dve", lambda e: e.memset(offt[:, 0, :], 0.0), [], [offt.r3(0, 0, 16)])
    for i in range(1, 16):
        P.add("dve", lambda e, i=i: e.tensor_tensor(out=offt[:, i, :], in0=offt[:, i - 1, :],
                                                    in1=tot_ps[:, (i - 1) * 16:i * 16], op=ALU.add),
              [offt.r3(i - 1, 0, 16), tot_ps.r((i - 1) * 16, i * 16)], [offt.r3(i, 0, 16)])
    P.add("dve", lambda e: e.tensor_tensor(out=rk[:], in0=offt[:].rearrange("p i e -> p (i e)"),
                                           in1=rank_ps[:, 0:256], op=ALU.add),
          [offt.r(), rank_ps.r(0, 256)], [rk.r()])
    P.add("dve", lambda e: e.scalar_tensor_tensor(out=rankm[:], in0=rk[:], scalar=1.0, in1=maskf[:],
                                                  op0=ALU.add, op1=ALU.mult), [rk.r(), maskf.r()], [rankm.r()])
    P.add("dve", lambda e: e.tensor_scalar(out=rankm[:], in0=rankm[:], scalar1=-1.0, scalar2=None, op0=ALU.add),
          [rankm.r()], [rankm.r()])
    if debug:
        dma("sp", "dbg_rankm", dbg["rankm"], rankm[:], [rankm.r()], [])

    if stop == "E":
        return finish()
    xn2_keys = [("dram", "xn2", i) for i in range(16)]
    acc_keys = [("dram", "acc", i) for i in range(16)]

    def s_build(e_):
        for i in range(16):
            P.add("dve", lambda e, i=i, e_=e_: e.tensor_scalar(out=Se[:, i, :], in0=iota256[:],
                                                               scalar1=rankm[:, i * 16 + e_:i * 16 + e_ + 1],
                                                               scalar2=None, op0=ALU.is_equal),
                  [iota256.r(), rankm.r(i * 16 + e_, i * 16 + e_ + 1)], [Se.r3(i, 0, 256)])

    def idx_gather(e_):
        for half in range(2):
            for i in range(16):
                P.add("pe", lambda e, i=i, half=half: e.matmul(pmisc[:, 32 + 2 * half:34 + 2 * half],
                                                               lhsT=Se[:, i, half * 128:(half + 1) * 128],
                                                               rhs=tinfob[:, i, :], start=(i == 0), stop=(i == 15)),
                      [Se.r3(i, half * 128, (half + 1) * 128), tinfob.r()],
                      [pmisc.r(32 + 2 * half, 34 + 2 * half)])
        P.add("dve", lambda e, e_=e_: e.tensor_reduce(out=idxf[:],
                                                      in_=pmisc[:, 32:36].rearrange("p (h t) -> p h t", t=2),
                                                      axis=AX.X, op=ALU.add),
              [pmisc.r(32, 36)], [idxf.r()])
        P.add("dve", lambda e, e_=e_: e.tensor_copy(out=idx_all[:, e_, :], in_=idxf[:]),
              [idxf.r()], [idx_all.r3(e_, 0, 2)])
        for half in range(2):
            P.add("pool", lambda e, half=half, e_=e_: e.indirect_dma_start(
                out=xg[:, half, :], out_offset=None, in_=xn2_dram[:, :],
                in_offset=bass.IndirectOffsetOnAxis(ap=idx_all[:, e_, half:half + 1], axis=0)),
                [idx_all.r3(e_, half, half + 1)] + xn2_keys, [xg.r3(half, 0, XROW)], dma=f"xg{half}")

    def xe_transpose(e_):
        for half in range(2):
            for k in range(8):
                P.add("pe", lambda e, k=k, half=half: e.transpose(
                    out=trps[:, k, :], in_=xg[:, half, k * 128:(k + 1) * 128], identity=identb[:]),
                    [xg.r3(half, k * 128, (k + 1) * 128), identb.r()], [trps.r3(k, 0, 128)])
            for k in range(8):
                if half == 0:
                    P.add("act", lambda e, k=k, half=half: e.activation(
                        out=xeT[:, k, half * 128:(half + 1) * 128], in_=trps[:, k, :], func=AF.Identity,
                        scale=gsc2x[:, k:k + 1], bias=mod1(24 + k, 0)),
                        [trps.r3(k, 0, 128), gsc2x.r(), modsb.r()], [xeT.r3(k, half * 128, (half + 1) * 128)])
                else:
                    P.add("dve", lambda e, k=k, half=half: e.tensor_scalar(
                        out=xeT[:, k, half * 128:(half + 1) * 128], in0=trps[:, k, :], scalar1=gsc2x[:, k:k + 1],
                        scalar2=mod1(24 + k, 0), op0=ALU.mult, op1=ALU.add),
                        [trps.r3(k, 0, 128), gsc2x.r(), modsb.r()], [xeT.r3(k, half * 128, (half + 1) * 128)])

    ps1q = (pbank[4], pbank[0])
    ps3q = (pbank[5], pbank[1])

    def expert_h13(e_):
        for cb in range(4):
            sa, b1 = acquire()
            sb_, b3 = acquire()
            for m in range(4):
                hm = cb * 4 + m
                q = m % 2
                ps1, ps3 = ps1q[q], ps3q[q]
                for (pb, bw) in ((ps1, b1), (ps3, b3)):
                    for k in range(8):
                        P.add("pe", lambda e, k=k, m=m, pb=pb, bw=bw: e.matmul(
                            pb[:, 0:256], lhsT=bw[:, k, m * 128:(m + 1) * 128], rhs=xeT[:, k, :],
                            start=(k == 0), stop=(k == 7)),
                            [bw.r3(k, m * 128, (m + 1) * 128), xeT.r3(k, 0, 256)],
                            [pb.r(0, 256)])
                slb = sl[q]
                P.add("act", lambda e, ps1=ps1, slb=slb: e.activation(out=slb[:], in_=ps1[:, 0:256],
                                                                      func=AF.Silu),
                      [ps1.r(0, 256)], [slb.r()])
                P.add("dve", lambda e, ps3=ps3, hm=hm, slb=slb: e.tensor_tensor(
                    out=hidT[:, hm, :], in0=slb[:], in1=ps3[:, 0:256], op=ALU.mult),
                    [slb.r(), ps3.r(0, 256)], [hidT.r3(hm, 0, 256)])
            release(sa)
            release(sb_)

    gate_all = mb("gate_all", [128, NE, 2], F32)

    def gate_copy(e_):
        for half in range(2):
            P.add("dve", lambda e, half=half, e_=e_: e.tensor_reduce(
                out=gate_all[:, e_, half:half + 1], in_=xg[:, half, D + 3 * e_:D + 3 * e_ + 3], axis=AX.X,
                op=ALU.add), [xg.r3(half, D, XROW)], [gate_all.r3(e_, half, half + 1)])

    def expert_w2_v2(e_):
        for kg in range(4):
            s2, b2 = acquire()
            for kk in range(4):
                k = kg * 4 + kk
                for half in range(2):
                    for n in range(2):
                        pb = pbank[half * 2 + n]
                        P.add("pe", lambda e, k=k, kk=kk, half=half, n=n, pb=pb, b2=b2: e.matmul(
                            pb[:, :], lhsT=hidT[:, k, half * 128:(half + 1) * 128],
                            rhs=b2[:, kk, n * 512:(n + 1) * 512], start=(k == 0), stop=(k == 15)),
                            [hidT.r3(k, half * 128, (half + 1) * 128), b2.r3(kk, n * 512, (n + 1) * 512)],
                            [pb.r()])
            release(s2)
        for half in range(2):
            for n in range(2):
                pb = pbank[half * 2 + n]
                P.add("dve", lambda e, half=half, n=n, pb=pb, e_=e_: e.scalar_tensor_tensor(
                    out=ye[:, half, n * 512:(n + 1) * 512], in0=pb[:, :], scalar=gate_all[:, e_, half:half + 1],
                    in1=g2x_rep[:, n * 512:(n + 1) * 512], op0=ALU.mult, op1=ALU.mult),
                    [pb.r(), gate_all.r3(e_, half, half + 1), g2x_rep.r(n * 512, (n + 1) * 512)],
                    [ye.r3(half, n * 512, (n + 1) * 512)])
        for half in range(2):
            P.add("pool", lambda e, half=half, e_=e_: e.indirect_dma_start(
                out=acc_dram[:, :], out_offset=bass.IndirectOffsetOnAxis(ap=idx_all[:, e_, half:half + 1], axis=0),
                in_=ye[:, half, :], in_offset=None, compute_op=ALU.add),
                [ye.r3(half, 0, D), idx_all.r3(e_, half, half + 1)] + acc_keys, acc_keys, dma=f"scat{half}")

    s_build(0)
    idx_gather(0)
    xe_transpose(0)
    gate_copy(0)
    s_build(1)
    idx_gather(1)
    for e_ in range(NE):
        expert_h13(e_)
        if e_ + 2 < NE:
            s_build(e_ + 2)
        if e_ + 1 < NE:
            xe_transpose(e_ + 1)
            gate_copy(e_ + 1)
        expert_w2_v2(e_)
        if e_ + 2 < NE:
            idx_gather(e_ + 2)

    if stop == "F":
        return finish()
    x2bufs = [x1t[0], x1t[1], x2e[0], x2e[1]]
    x2grp = ["x1t0", "x1t1", "x2e0", "x2e1"]
    for i in range(16):
        par = i % 2
        x2, ot = x2bufs[i % 4], outt[i % 4]
        junk = xn2b[par]
        if i == 0:
            for j in range(3):
                dma("sp", x2grp[j % 4], x2bufs[j % 4][:], acc_dram[j * 128:(j + 1) * 128, :], acc_keys,
                    [x2bufs[j % 4].r()])
        if i + 3 < 16:
            j = i + 3
            dma("sp", x2grp[j % 4], x2bufs[j % 4][:], acc_dram[j * 128:(j + 1) * 128, :], acc_keys,
                [x2bufs[j % 4].r()])
        P.add("act", lambda e, i=i, x2=x2, junk=junk: e.activation(out=junk[:, 0:D], in_=x2[:], func=AF.Square,
                                                                   accum_out=ss3[:, i:i + 1]),
              [x2.r()], [junk.r(0, D), ss3.r(i, i + 1)])
        rstd_op(rstd3, ss3, i, i + 1, 1.0 / D)
        if i % 2 == 0:
            P.add("dve", lambda e, i=i, x2=x2, ot=ot: e.scalar_tensor_tensor(
                out=ot[:], in0=x2[:], scalar=rstd3[:, i:i + 1], in1=fg_rep[:], op0=ALU.mult, op1=ALU.mult),
                [x2.r(), rstd3.r(i, i + 1), fg_rep.r()], [ot.r()])
        else:
            P.add("act", lambda e, i=i, x2=x2, ot=ot: e.activation(out=ot[:], in_=x2[:], func=AF.Copy,
                                                                   scale=rstd3[:, i:i + 1]),
                  [x2.r(), rstd3.r(i, i + 1)], [ot.r()])
            P.add("pool", lambda e, ot=ot: e.tensor_tensor(out=ot[:], in0=ot[:], in1=fg_rep[:], op=ALU.mult),
                  [ot.r(), fg_rep.r()], [ot.r()])
        dma("sp", f"outw{i % 4}", out_d[i * 128:(i + 1) * 128, :], ot[:], [ot.r()], [("dram", "out", i)])

    with ExitStack() as stack:
        P.emit(stack)
    return nc


_CACHE = {}


def _fm(v, k):
    return np.ascontiguousarray(np.asarray(v, np.float32).reshape(k, 128).T)


def make_in_maps(x, c, ctx, c_ctx, w_mod, b_mod, norm1_g, norm2_g, w_in, sgu_g, sgu_w, sgu_b, conv_w, conv_b,
                 rg_wa, rg_ba, rg_wi, rg_bi, rg_lam, w_out, w_router, w1, w3, w2, final_g):
    f = lambda a: np.ascontiguousarray(np.asarray(a, dtype=np.float32))
    x, c, ctx, c_ctx = f(x), f(c), f(ctx), f(c_ctx)
    shared = {}
    shared["w_mod"] = f(w_mod[0])
    shared["bmod"] = _fm(b_mod[0], 48)
    shared["n1g"] = _fm(norm1_g[0], 8)
    shared["n2g"] = _fm(norm2_g[0], 8)
    shared["w_in"] = f(w_in[0])
    shared["sgug_rep"] = np.ascontiguousarray(np.broadcast_to(f(sgu_g[0])[None, :], (128, 512)))
    shared["wsT"] = np.ascontiguousarray(np.transpose(f(sgu_w[0]), (2, 0, 1)))
    shared["sgub"] = f(sgu_b[0]).reshape(1, 512)
    cw = f(conv_w[0])
    shared["convw"] = np.ascontiguousarray(np.transpose(cw.reshape(4, 4, 128), (2, 1, 0)))
    shared["convb"] = _fm(conv_b[0], 4)
    wg = np.zeros((128, 16, 128), np.float32)
    gb = np.zeros((128, 16), np.float32)
    lam = np.zeros((128, 8), np.float32)
    wa, wi = f(rg_wa[0]), f(rg_wi[0])
    ba, bi, lm = f(rg_ba[0]), f(rg_bi[0]), f(rg_lam[0])
    for d in range(2):
        for cc in range(4):
            for g, (w_, b_) in enumerate(((wa, ba), (wi, bi))):
                idx = (d * 2 + g) * 4 + cc
                for hh in range(2):
                    wg[hh * 64:(hh + 1) * 64, idx, hh * 64:(hh + 1) * 64] = w_[d, 2 * cc + hh]
                gb[:, idx] = b_[d, cc * 128:(cc + 1) * 128]
            lam[:, d * 4 + cc] = lm[d, cc * 128:(cc + 1) * 128]
    shared["wg"], shared["gb"], shared["lam"] = wg, gb, lam
    shared["w_out"] = f(w_out[0])
    shared["wr"] = np.ascontiguousarray(f(w_router[0]).reshape(8, 128, 16).transpose(1, 0, 2))
    shared["w1"], shared["w3"], shared["w2"] = f(w1[0]), f(w3[0]), f(w2[0])
    shared["fg_rep"] = np.ascontiguousarray(np.broadcast_to(f(final_g)[None, :], (128, D)))
    shared["ident"] = np.eye(128, dtype=np.float32)
    shared["iota256"] = np.ascontiguousarray(np.broadcast_to(np.arange(256, dtype=np.float32)[None, :], (128, 256)))
    kk, mm = np.meshgrid(np.arange(128), np.arange(128), indexing="ij")
    shared["ltri"] = (kk < mm).astype(np.float32)
    shared["blk"] = ((kk // 8) == (mm // 8)).astype(np.float32)
    sel = np.zeros((128, 16, 16), np.float32)
    for e in range(16):
        sel[8 * e, :, e] = 1.0
    shared["selfull"] = sel.reshape(128, 256)
    ti = np.zeros((128, 16, 2), np.float32)
    ti[:, :, 0] = np.arange(128)[:, None]
    ti[:, :, 1] = (np.arange(16) * 128)[None, :]
    shared["tinfo"] = ti
    cctx_fm = _fm(c_ctx, 8)
    in_maps = []
    for b in range(NCORES):
        m = dict(shared)
        m["x"] = x[b]
        m["ctx"] = ctx[b]
        cv = np.zeros((128, 8, 2), np.float32)
        cv[:, :, 0] = _fm(c[b], 8)
        cv[:, :, 1] = cctx_fm
        m["cvec"] = cv
        in_maps.append(m)
    return in_maps


def kernel(**inputs):
    if "nc" not in _CACHE:
        _CACHE["nc"] = build_program()
    nc = _CACHE["nc"]
    in_maps = make_in_maps(**inputs)
    res = run_bass_kernel_spmd(nc, in_maps, core_ids=list(range(NCORES)))
    out = np.stack([np.asarray(r["out"], dtype=np.float32) for r in res.results], axis=0)
    return out
```
